# Optimizing a Trainium2 kernel written in Bass

```python
import math
import jax, jax.numpy as jnp
from jax import lax
import numpy as np

D_MODEL = 1024
BATCH = 8
SEQ = 2048
DEPTH = 2

GRID_W = 64
D_MIX = 256
N_BRANCH = 4
EPS = 1e-6
HY_ORDER = 2
HY_SHORT = 3
HY_BANDS = 16
HY_EMB = 2 * HY_BANDS + 1
HY_FFN = 64
HY_TARGET = 1e-2
HY_FAST_DECAY = 0.3
HY_SLOW_DECAY = 1.5
FN_GROUPS = 4
MLA_HEADS = 4
MLA_NOPE = 64
MLA_ROPE = 32
MLA_V = 64
MLA_Q_RANK = 256
MLA_KV_RANK = 128
ROPE_THETA = 10000.0
Q_BLOCK = 128
NA_HEADS = 4
NA_HEAD_DIM = D_MIX // NA_HEADS
NA_WIN_R = 8
NA_WIN_C = 16
N_EXPERTS = 16
EC_CAPACITY_FACTOR = 2
D_FF_EXPERT = 1024
PLE_DIM = 256

HY_COLS = 3 * D_MIX
FN_COLS = D_MIX
NA_COLS = 3 * D_MIX
GATE_COLS = N_BRANCH * D_MODEL
IN_SIZES = (HY_COLS, FN_COLS, MLA_Q_RANK, MLA_KV_RANK, MLA_ROPE, NA_COLS, GATE_COLS)
IN_COLS = sum(IN_SIZES)

kernel_name = "hybrid_hyena_fnet_mla_natten_ec_moe_encoder"


def rmsnorm(x, g):
    xf = x.astype(jnp.float32)
    y = xf * lax.rsqrt(jnp.mean(xf * xf, axis=-1, keepdims=True) + EPS)
    return (y * g.astype(jnp.float32)).astype(x.dtype)


def hyena_filters(L, w1, b1, freq, w2, b2, w3):
    f32 = jnp.float32
    t01 = jnp.linspace(0.0, 1.0, L, dtype=f32)[:, None]
    bands = jnp.linspace(1e-4, HY_BANDS - 1, HY_BANDS, dtype=f32)
    ang = 2.0 * math.pi * jnp.arange(L, dtype=f32)[:, None] * bands / L
    z = jnp.concatenate([t01, jnp.cos(ang), -jnp.sin(ang)], axis=-1)
    freq = freq.astype(f32)
    hf = jnp.sin(freq[0] * (z @ w1.astype(f32) + b1.astype(f32)))
    hf = jnp.sin(freq[1] * (hf @ w2.astype(f32) + b2.astype(f32)))
    hf = hf @ w3.astype(f32)
    max_decay = math.log(HY_TARGET) / HY_FAST_DECAY
    min_decay = math.log(HY_TARGET) / HY_SLOW_DECAY
    deltas = jnp.linspace(min_decay, max_decay, D_MIX, dtype=f32)
    window = jnp.exp(-t01 * jnp.abs(deltas))
    hf = hf.reshape(L, HY_ORDER, 2, D_MIX) * window[:, None, None, :]
    fwd, bwd = hf[:, :, 0], hf[:, :, 1]
    k = jnp.concatenate([fwd, jnp.zeros((1, HY_ORDER, D_MIX), f32), bwd[1:][::-1]], axis=0)
    k = k * lax.rsqrt(jnp.sum(k * k, axis=0, keepdims=True) + EPS)
    return k.transpose(1, 0, 2)


def fft_long_conv(z, k, skip):
    L = z.shape[1]
    zf = jnp.fft.rfft(z, n=2 * L, axis=1)
    kf = jnp.fft.rfft(k, axis=0)
    y = jnp.fft.irfft(zf * kf, n=2 * L, axis=1)[:, :L]
    return y + z * skip.astype(jnp.float32)


def hyena_branch(u, conv_w, conv_b, w1, b1, freq, w2, b2, w3, skip):
    L = u.shape[1]
    up = jnp.pad(u, ((0, 0), (1, 1), (0, 0)))
    uc = up[:, :-2] * conv_w[0] + up[:, 1:-1] * conv_w[1] + up[:, 2:] * conv_w[2] + conv_b
    x1, x2, v = jnp.split(uc.astype(jnp.float32), 3, axis=-1)
    filt = hyena_filters(L, w1, b1, freq, w2, b2, w3)
    z = x1 * fft_long_conv(v, filt[0], skip[0])
    z = x2 * fft_long_conv(z, filt[1], skip[1])
    return z.astype(u.dtype)


def fnet_branch(u):
    B, L, _ = u.shape
    ug = u.astype(jnp.float32).reshape(B, L, FN_GROUPS, D_MIX // FN_GROUPS)
    y = jnp.fft.fft2(ug, axes=(1, 3), norm="ortho").real
    return y.reshape(B, L, D_MIX).astype(u.dtype)


def rope_tables(L):
    inv = ROPE_THETA ** (-jnp.arange(0, MLA_ROPE, 2, dtype=jnp.float32) / MLA_ROPE)
    ang = jnp.arange(L, dtype=jnp.float32)[:, None] * inv
    return jnp.cos(ang), jnp.sin(ang)


def apply_rope(x, cos, sin):
    x1, x2 = jnp.split(x, 2, axis=-1)
    return jnp.concatenate([x1 * cos - x2 * sin, x2 * cos + x1 * sin], axis=-1).astype(x.dtype)


def mla_branch(c_q_raw, c_kv_raw, k_pe_raw, q_norm_g, w_uq, kv_norm_g, w_ukv):
    B, L, _ = c_q_raw.shape
    H = MLA_HEADS
    cos, sin = rope_tables(L)
    q = (rmsnorm(c_q_raw, q_norm_g) @ w_uq).reshape(B, L, H, MLA_NOPE + MLA_ROPE)
    q_nope, q_pe = q[..., :MLA_NOPE], apply_rope(q[..., MLA_NOPE:], cos[:, None], sin[:, None])
    kv = (rmsnorm(c_kv_raw, kv_norm_g) @ w_ukv).reshape(B, L, H, MLA_NOPE + MLA_V)
    k_nope, v = kv[..., :MLA_NOPE], kv[..., MLA_NOPE:]
    k_pe = apply_rope(k_pe_raw, cos, sin)
    scale = (MLA_NOPE + MLA_ROPE) ** -0.5
    nb = L // Q_BLOCK

    def block(args):
        qn, qp = args
        s = jnp.einsum('bqhd,bkhd->bhqk', qn, k_nope) + jnp.einsum('bqhr,bkr->bhqk', qp, k_pe)
        pr = jax.nn.softmax(s.astype(jnp.float32) * scale, axis=-1).astype(v.dtype)
        return jnp.einsum('bhqk,bkhd->bqhd', pr, v)

    qn_b = q_nope.reshape(B, nb, Q_BLOCK, H, MLA_NOPE).swapaxes(0, 1)
    qp_b = q_pe.reshape(B, nb, Q_BLOCK, H, MLA_ROPE).swapaxes(0, 1)
    o = lax.map(block, (qn_b, qp_b))
    return o.swapaxes(0, 1).reshape(B, L, H * MLA_V)


def neighborhood_branch(u, rpb):
    B, L, _ = u.shape
    H, d = NA_HEADS, NA_HEAD_DIM
    R = L // GRID_W
    wr = min(NA_WIN_R, R)
    q, k, v = jnp.split(u, 3, axis=-1)
    r = jnp.arange(R)
    rows = jnp.clip(r - wr // 2, 0, R - wr)[:, None] + jnp.arange(wr)
    c = jnp.arange(GRID_W)
    cs = jnp.clip(c - NA_WIN_C // 2, 0, GRID_W - NA_WIN_C)
    col_mask = (c[None, :] >= cs[:, None]) & (c[None, :] < cs[:, None] + NA_WIN_C)
    qg = q.reshape(B, R, GRID_W, H, d)
    kg = k.reshape(B, R, GRID_W, H, d)[:, rows]
    vg = v.reshape(B, R, GRID_W, H, d)[:, rows]
    s = jnp.einsum('brqhd,brikhd->bhrqik', qg, kg).astype(jnp.float32) * (d ** -0.5)
    dr = rows - r[:, None] + (NA_WIN_R - 1)
    dc = jnp.clip(c[None, :] - c[:, None] + (NA_WIN_C - 1), 0, 2 * NA_WIN_C - 2)
    bias = rpb.astype(jnp.float32)[:, dr][..., dc]
    s = s + bias.transpose(0, 1, 3, 2, 4)[None]
    s = jnp.where(col_mask[:, None, :], s, -jnp.inf)
    pr = jax.nn.softmax(s, axis=(-2, -1)).astype(u.dtype)
    o = jnp.einsum('bhrqik,brikhd->brqhd', pr, vg)
    return o.reshape(B, L, H * d)


def expert_choice_ffn(h, w_router, w_gate, w_up, w_down):
    B, L, D = h.shape
    cap = EC_CAPACITY_FACTOR * L // N_EXPERTS
    logits = jnp.einsum('bld,de->ble', h, w_router).astype(jnp.float32)
    aff = jax.nn.softmax(logits, axis=-1)
    top_aff, top_idx = lax.top_k(aff.transpose(0, 2, 1), cap)
    xe = jax.vmap(lambda hb, ib: hb[ib])(h, top_idx)
    g = jnp.einsum('becd,edf->becf', xe, w_gate)
    up = jnp.einsum('becd,edf->becf', xe, w_up)
    ye = jnp.einsum('becf,efd->becd', jax.nn.silu(g) * up, w_down)
    ye = ye * top_aff[..., None].astype(h.dtype)
    return jnp.zeros_like(h).at[jnp.arange(B)[:, None, None], top_idx].add(ye)


def mixer_sublayer(x, norm1_g, w_in, b_gate, hy_conv_w, hy_conv_b, hf_w1, hf_b1, hf_freq,
                   hf_w2, hf_b2, hf_w3, hy_skip, q_norm_g, w_uq, kv_norm_g, w_ukv, rpb,
                   w_br, w_out):
    B, L, D = x.shape
    h = rmsnorm(x, norm1_g)
    u = h @ w_in
    offsets = np.cumsum(IN_SIZES)[:-1].tolist()
    u_hy, u_fn, u_cq, u_ckv, u_kpe, u_na, u_gate = jnp.split(u, offsets, axis=-1)
    y_hy = hyena_branch(u_hy, hy_conv_w, hy_conv_b, hf_w1, hf_b1, hf_freq, hf_w2, hf_b2, hf_w3, hy_skip)
    y_fn = fnet_branch(u_fn)
    y_mla = mla_branch(u_cq, u_ckv, u_kpe, q_norm_g, w_uq, kv_norm_g, w_ukv)
    y_na = neighborhood_branch(u_na, rpb)
    branches = jnp.stack([y_hy, y_fn, y_mla, y_na], axis=2)
    proj = jnp.einsum('blnc,ncd->blnd', branches, w_br)
    gates = jax.nn.sigmoid(u_gate + b_gate).reshape(B, L, N_BRANCH, D)
    merged = jnp.sum(gates * proj, axis=2)
    return x + merged @ w_out


def setup_inputs(seed: int = 0) -> dict:
    key = jax.random.key(seed)
    ks = jax.random.split(key, 30)
    f32 = jnp.float32

    def nrm(k, shape, scale):
        return jax.random.normal(k, shape, f32) * scale

    def gain(k, shape):
        return 1.0 + 0.01 * jax.random.normal(k, shape, f32)

    return {
        "x": nrm(ks[0], (BATCH, SEQ, D_MODEL), 1.0),
        "p": nrm(ks[1], (DEPTH, BATCH, SEQ, PLE_DIM), 1.0),
        "norm1_g": gain(ks[2], (DEPTH, D_MODEL)),
        "w_in": nrm(ks[3], (DEPTH, D_MODEL, IN_COLS), D_MODEL ** -0.5),
        "b_gate": nrm(ks[4], (DEPTH, GATE_COLS), 0.02),
        "hy_conv_w": nrm(ks[5], (DEPTH, HY_SHORT, HY_COLS), HY_SHORT ** -0.5),
        "hy_conv_b": nrm(ks[6], (DEPTH, HY_COLS), 0.02),
        "hf_w1": nrm(ks[7], (DEPTH, HY_EMB, HY_FFN), HY_EMB ** -0.5),
        "hf_b1": nrm(ks[8], (DEPTH, HY_FFN), 0.02),
        "hf_freq": gain(ks[9], (DEPTH, 2, HY_FFN)),
        "hf_w2": nrm(ks[10], (DEPTH, HY_FFN, HY_FFN), HY_FFN ** -0.5),
        "hf_b2": nrm(ks[11], (DEPTH, HY_FFN), 0.02),
        "hf_w3": nrm(ks[12], (DEPTH, HY_FFN, HY_ORDER * 2 * D_MIX), HY_FFN ** -0.5),
        "hy_skip": nrm(ks[13], (DEPTH, HY_ORDER, D_MIX), 0.5),
        "q_norm_g": gain(ks[14], (DEPTH, MLA_Q_RANK)),
        "w_uq": nrm(ks[15], (DEPTH, MLA_Q_RANK, MLA_HEADS * (MLA_NOPE + MLA_ROPE)), MLA_Q_RANK ** -0.5),
        "kv_norm_g": gain(ks[16], (DEPTH, MLA_KV_RANK)),
        "w_ukv": nrm(ks[17], (DEPTH, MLA_KV_RANK, MLA_HEADS * (MLA_NOPE + MLA_V)), MLA_KV_RANK ** -0.5),
        "rpb": nrm(ks[18], (DEPTH, NA_HEADS, 2 * NA_WIN_R - 1, 2 * NA_WIN_C - 1), 0.02),
        "w_br": nrm(ks[19], (DEPTH, N_BRANCH, D_MIX, D_MODEL), D_MIX ** -0.5),
        "w_out": nrm(ks[20], (DEPTH, D_MODEL, D_MODEL), D_MODEL ** -0.5),
        "norm2_g": gain(ks[21], (DEPTH, D_MODEL)),
        "w_router": nrm(ks[22], (DEPTH, D_MODEL, N_EXPERTS), D_MODEL ** -0.5),
        "w_e_gate": nrm(ks[23], (DEPTH, N_EXPERTS, D_MODEL, D_FF_EXPERT), D_MODEL ** -0.5),
        "w_e_up": nrm(ks[24], (DEPTH, N_EXPERTS, D_MODEL, D_FF_EXPERT), D_MODEL ** -0.5),
        "w_e_down": nrm(ks[25], (DEPTH, N_EXPERTS, D_FF_EXPERT, D_MODEL), D_FF_EXPERT ** -0.5),
        "norm3_g": gain(ks[26], (DEPTH, D_MODEL)),
        "w_ple_gate": nrm(ks[27], (DEPTH, D_MODEL, D_MODEL), D_MODEL ** -0.5),
        "w_ple_proj": nrm(ks[28], (DEPTH, PLE_DIM, D_MODEL), PLE_DIM ** -0.5),
        "final_g": gain(ks[29], (D_MODEL,)),
    }


def reference(x, p, norm1_g, w_in, b_gate, hy_conv_w, hy_conv_b, hf_w1, hf_b1, hf_freq,
              hf_w2, hf_b2, hf_w3, hy_skip, q_norm_g, w_uq, kv_norm_g, w_ukv, rpb, w_br,
              w_out, norm2_g, w_router, w_e_gate, w_e_up, w_e_down, norm3_g, w_ple_gate,
              w_ple_proj, final_g):
    for i in range(DEPTH):
        x = mixer_sublayer(x, norm1_g[i], w_in[i], b_gate[i], hy_conv_w[i], hy_conv_b[i],
                           hf_w1[i], hf_b1[i], hf_freq[i], hf_w2[i], hf_b2[i], hf_w3[i],
                           hy_skip[i], q_norm_g[i], w_uq[i], kv_norm_g[i], w_ukv[i], rpb[i],
                           w_br[i], w_out[i])
        x = x + expert_choice_ffn(rmsnorm(x, norm2_g[i]), w_router[i], w_e_gate[i],
                                  w_e_up[i], w_e_down[i])
        gate = jax.nn.sigmoid(rmsnorm(x, norm3_g[i]) @ w_ple_gate[i])
        x = x + gate * (p[i] @ w_ple_proj[i])
    return rmsnorm(x, final_g)
```

```python
import math
from contextlib import ExitStack
import numpy as np
import ml_dtypes
import concourse.bass as bass
import concourse.mybir as mybir
from concourse.bass_utils import run_bass_kernel_spmd

F32 = mybir.dt.float32
BF16 = mybir.dt.bfloat16
ALU = mybir.AluOpType
AF = mybir.ActivationFunctionType
AX = mybir.AxisListType

L = 2048
D = 1024
NT = 16
DEPTH = 2
EPS = 1e-6
IN_COLS = 6304
O_HY, O_FN, O_CQ, O_CKV, O_KPE, O_NA, O_GATE = 0, 768, 1024, 1280, 1408, 1440, 2208
NFFT = 4096


class KB:
    N_DMA_SEMS = 28
    N_HW = 20

    def __init__(self, nc, same_engine_sync=True):
        self.nc = nc
        self.eng = {"pe": nc.tensor, "act": nc.scalar, "dve": nc.vector,
                    "pool": nc.gpsimd, "sp": nc.sync}
        self.sem = {k: nc.alloc_semaphore("s_" + k) for k in ("pe", "act", "dve", "pool")}
        self.cnt = {k: 0 for k in self.sem}
        self.pending = {k: [] for k in self.sem}
        self.seen = {k: {} for k in self.eng}
        self.dsem = [nc.alloc_semaphore("d%d" % i) for i in range(self.N_DMA_SEMS)]
        self.dval = [0] * self.N_DMA_SEMS
        self.drr = 0
        self.drr_sw = 0
        self.res = {}
        self.ses = same_engine_sync
        self.n_ins = 0
        self.n_wait = 0
        self.track = None
        self.phase = ""
        self.excl = set()

    def _r(self, key):
        r = self.res.get(key)
        if r is None:
            r = {"w": None, "r": []}
            self.res[key] = r
        return r

    def _collect(self, reads, writes, ename=None):
        st = []
        for k in reads:
            r = self._r(k)
            if r["w"] is not None:
                st.append(r["w"])
        for k in writes:
            r = self._r(k)
            if r["w"] is not None:
                st.append(r["w"])
            st.extend(r["r"])
        return st

    def _emit_waits(self, ename, stamps, skip_same=False):
        need = {}
        for (kind, idx, val) in stamps:
            if kind == "e":
                if idx == ename and (skip_same or not self.ses):
                    continue
                assert val <= self.cnt[idx], "dependency on pending stamp %s %d" % (idx, val)
            key = (kind, idx)
            if self.seen[ename].get(key, 0) >= val:
                continue
            if need.get(key, 0) < val:
                need[key] = val
        e = self.eng[ename]
        for (kind, idx), val in need.items():
            s = self.sem[idx] if kind == "e" else self.dsem[idx]
            e.wait_ge(s, val)
            self.seen[ename][(kind, idx)] = val
            self.n_wait += 1

    def _stamp(self, reads, writes, stamp):
        for k in reads:
            rr = self._r(k)["r"]
            rr.append(stamp)
            if len(rr) > 64:
                best = {}
                for s in rr:
                    kk = (s[0], s[1])
                    if best.get(kk, (0, 0, 0))[2] < s[2]:
                        best[kk] = s
                rr[:] = list(best.values())
        for k in writes:
            r = self._r(k)
            r["w"] = stamp
            r["r"] = []

    def op(self, ename, fn, reads=(), writes=(), inc=True, skip_same=False):
        reads = list(reads); writes = list(writes)
        writes = writes + [r for r in reads if r in self.excl and r not in writes]
        self._emit_waits(ename, self._collect(reads, writes, ename), skip_same=skip_same)
        ins = fn(self.eng[ename])
        self.n_ins += 1
        if self.track is not None:
            try:
                self.track[str(ins.ins.name)] = self.phase
            except Exception:
                pass
        if inc:
            ins.then_inc(self.sem[ename], 1)
            self.cnt[ename] += 1
            stamp = ("e", ename, self.cnt[ename])
            for (rd, wr) in self.pending[ename]:
                self._stamp(rd, wr, stamp)
            self.pending[ename] = []
            self._stamp(reads, writes, stamp)
        else:
            self.pending[ename].append((reads, writes))
        return ins

    def dma(self, qname, out, in_, reads=(), writes=(), **kw):
        reads = list(reads); writes = list(writes)
        if qname == "pool":
            i = self.N_HW + self.drr_sw
            self.drr_sw = (self.drr_sw + 1) % (self.N_DMA_SEMS - self.N_HW)
        else:
            i = self.drr
            self.drr = (self.drr + 1) % self.N_HW
        st = self._collect(reads, writes)
        if self.dval[i] > 0:
            st.append(("d", i, self.dval[i]))
        self._emit_waits(qname, st, skip_same=True)
        ins = self.eng[qname].dma_start(out=out, in_=in_, **kw)
        self.n_ins += 1
        self.dval[i] += 16
        ins.then_inc(self.dsem[i], 16)
        self._stamp(reads, writes, ("d", i, self.dval[i]))
        return ins

    def barrier(self):
        st = [("e", k, v) for k, v in self.cnt.items() if v > 0]
        st += [("d", i, v) for i, v in enumerate(self.dval) if v > 0]
        for k in self.pending:
            assert not self.pending[k]
        for ename in self.eng:
            self._emit_waits(ename, [s for s in st if not (s[0] == "e" and s[1] == ename)])
        self.res = {}

    def finish(self, qname="sp"):
        st = [("e", k, v) for k, v in self.cnt.items() if v > 0]
        st += [("d", i, v) for i, v in enumerate(self.dval) if v > 0]
        self._emit_waits(qname, st)


class Rot:
    def __init__(self, tiles, name):
        self.tiles = tiles
        self.name = name
        self.i = 0

    def next(self):
        j = self.i % len(self.tiles)
        self.i += 1
        return self.tiles[j], "%s#%d" % (self.name, j)


class Ctx:
    def __init__(self, nc, k, tag):
        self.nc, self.k, self.tag = nc, k, tag
        self.es = ExitStack()
        self.n = 0

    def __enter__(self):
        self.es.__enter__()
        self.prev_phase = self.k.phase
        self.k.phase = self.tag
        return self

    def __exit__(self, *a):
        if a[0] is None:
            self.k.barrier()
        self.k.phase = self.prev_phase
        return self.es.__exit__(*a)

    def sb(self, shape, dt, name=None):
        self.n += 1
        return self.es.enter_context(self.nc.sbuf_tensor("%s_%s%d" % (self.tag, name or "t", self.n), list(shape), dt))

    def ps(self, shape, dt, name=None):
        self.n += 1
        esz = 2 if dt == BF16 else 4
        n = 1
        for d in shape[1:]:
            n *= d
        assert n * esz <= 2048, shape
        full = self.es.enter_context(self.nc.psum_tensor("%s_%s%d" % (self.tag, name or "p", self.n), [128, 2048 // esz], dt))
        v = full[0:shape[0], 0:n]
        if len(shape) == 3:
            v = v.rearrange("p (a b) -> p a b", a=shape[1])
        return v

    def rot_sb(self, n, shape, dt, name):
        return Rot([self.sb(shape, dt, name) for _ in range(n)], self.tag + name)

    def rot_ps(self, n, shape, dt, name):
        r = Rot([self.ps(shape, dt, name) for _ in range(n)], self.tag + name)
        for j in range(n):
            self.k.excl.add("%s#%d" % (r.name, j))
        return r


def host_consts():
    f32 = np.float32
    bf = ml_dtypes.bfloat16
    c = {}
    t = np.arange(L, dtype=np.int64)
    m = (np.outer(t, t) % NFFT).astype(np.float64) * (2.0 * np.pi / NFFT)
    c["dftA"] = np.cos(m).astype(bf)
    c["dftB"] = np.sin(m).astype(bf)
    c["ident"] = np.eye(128, dtype=f32)
    sg = np.where(np.arange(128) % 2 == 0, 1.0, -1.0)
    c["signp"] = sg.astype(f32).reshape(128, 1)
    c["altcol"] = sg.astype(f32).reshape(128, 1)
    c["altrow"] = np.where(t % 2 == 0, 1.0, -1.0).astype(f32).reshape(1, L)
    t01 = np.linspace(0.0, 1.0, L, dtype=f32)[:, None]
    bands = np.linspace(1e-4, 15, 16, dtype=f32)
    ang = f32(2.0 * math.pi) * np.arange(L, dtype=f32)[:, None] * bands / f32(L)
    z = np.concatenate([t01, np.cos(ang), -np.sin(ang)], axis=-1).astype(f32)
    c["hy_zT"] = np.ascontiguousarray(z.T)
    max_decay = math.log(1e-2) / 0.3
    min_decay = math.log(1e-2) / 1.5
    deltas = np.linspace(min_decay, max_decay, 256, dtype=f32)
    c["hy_win"] = np.exp(-t01 * np.abs(deltas)).astype(f32)
    inv = (10000.0 ** (-np.arange(0, 32, 2, dtype=f32) / f32(32))).astype(f32)
    ra = np.arange(L, dtype=f32)[:, None] * inv
    rc_, rs_ = np.cos(ra).astype(f32), np.sin(ra).astype(f32)
    cs2 = np.concatenate([rc_, rc_], axis=1)
    sn2 = np.concatenate([-rs_, rs_], axis=1)
    c["rope_cs2"] = np.ascontiguousarray(cs2.reshape(NT, 128, 32).transpose(1, 0, 2))
    c["rope_sn2"] = np.ascontiguousarray(sn2.reshape(NT, 128, 32).transpose(1, 0, 2))
    qs = f32(96.0 ** -0.5)
    c["rope_cs2q"] = np.ascontiguousarray(np.repeat((cs2 * qs).reshape(NT, 128, 1, 32), 4, axis=2).transpose(1, 0, 2, 3))
    c["rope_sn2q"] = np.ascontiguousarray(np.repeat((sn2 * qs).reshape(NT, 128, 1, 32), 4, axis=2).transpose(1, 0, 2, 3))
    cc = np.arange(64)
    a64 = 2.0 * np.pi * (np.outer(cc, cc) % 64) / 64.0
    nrm = 1.0 / math.sqrt(L * 64.0)
    C = np.zeros((256, 256)); S = np.zeros((256, 256))
    for g in range(4):
        C[g * 64:(g + 1) * 64, g * 64:(g + 1) * 64] = np.cos(a64) * nrm
        S[g * 64:(g + 1) * 64, g * 64:(g + 1) * 64] = -np.sin(a64) * nrm
    c["fn_C"] = C.astype(f32)
    c["fn_S"] = S.astype(f32)
    E = np.zeros((32, 64, 64), dtype=f32)
    kc = np.arange(64)[:, None]; qc = np.arange(64)[None, :]
    dc = np.clip(kc - qc + 15, 0, 30)
    for b in range(31):
        E[b] = (dc == b)
    cs = np.clip(qc - 8, 0, 48)
    ok = (kc >= cs) & (kc < cs + 16)
    E[31] = np.where(ok, 0.0, -30000.0)
    c["na_E"] = E.reshape(32, 4096)
    c["iota256"] = np.tile(np.arange(256, dtype=f32)[None, :], (128, 1))
    c["iotap"] = np.stack([np.arange(128), np.arange(128) + 128], axis=1).astype(f32)
    sel = np.zeros((16, 16, 128), dtype=f32)
    for e in range(16):
        sel[e, e, :] = 1.0
    c["sel16"] = sel
    c["ones_f"] = np.ones((128, 128), dtype=f32)
    return c


CONST_SPECS = None


def const_specs():
    global CONST_SPECS
    if CONST_SPECS is None:
        CONST_SPECS = host_consts()
    return CONST_SPECS


WEIGHT_SHAPES = {
    "norm1_g": (2, 1024), "w_in": (2, 1024, 6304), "b_gate": (2, 4096), "hy_conv_w": (2, 3, 768),
    "hy_conv_b": (2, 768), "hf_w1": (2, 33, 64), "hf_b1": (2, 64), "hf_freq": (2, 2, 64),
    "hf_w2": (2, 64, 64), "hf_b2": (2, 64), "hf_w3": (2, 64, 1024), "hy_skip": (2, 2, 256),
    "q_norm_g": (2, 256), "w_uq": (2, 256, 384), "kv_norm_g": (2, 128), "w_ukv": (2, 128, 512),
    "rpb": (2, 4, 15, 31), "w_br": (2, 4, 256, 1024), "w_out": (2, 1024, 1024), "norm2_g": (2, 1024),
    "w_router": (2, 1024, 16), "w_e_gate": (2, 16, 1024, 1024), "w_e_up": (2, 16, 1024, 1024),
    "w_e_down": (2, 16, 1024, 1024), "norm3_g": (2, 1024), "w_ple_gate": (2, 1024, 1024),
    "w_ple_proj": (2, 256, 1024), "final_g": (1024,),
}


def mm(k, out, lhsT, rhs, start, stop, reads, writes, inc=True):
    return k.op("pe", lambda e: e.matmul(out, lhsT=lhsT, rhs=rhs, start=start, stop=stop), reads, writes, inc=inc)


def tp(k, out, in_, ident, reads, writes, inc=True):
    return k.op("pe", lambda e: e.transpose(out=out, in_=in_, identity=ident), reads, writes, inc=inc)


def cp(k, eng, out, in_, reads, writes):
    if eng == "act":
        return k.op("act", lambda e: e.copy(out=out, in_=in_), reads, writes)
    return k.op(eng, lambda e: e.tensor_copy(out=out, in_=in_), reads, writes)


def tt(k, eng, out, in0, in1, op, reads, writes):
    return k.op(eng, lambda e: e.tensor_tensor(out=out, in0=in0, in1=in1, op=op), reads, writes)


def ts(k, eng, out, in0, s1, s2, op0, op1, reads, writes):
    if op1 is None:
        return k.op(eng, lambda e: e.tensor_scalar(out=out, in0=in0, scalar1=s1, scalar2=None, op0=op0), reads, writes)
    return k.op(eng, lambda e: e.tensor_scalar(out=out, in0=in0, scalar1=s1, scalar2=s2, op0=op0, op1=op1), reads, writes)


def stt(k, out, in0, scalar, in1, op0, op1, reads, writes):
    return k.op("dve", lambda e: e.scalar_tensor_tensor(out=out, in0=in0, scalar=scalar, in1=in1, op0=op0, op1=op1), reads, writes)


def act(k, out, in_, func, reads, writes, **kw):
    return k.op("act", lambda e: e.activation(out=out, in_=in_, func=func, **kw), reads, writes)


def rms_rstd(k, src, skey, junk, jkey, ss, sskey, n):
    act(k, junk, src, AF.Square, [skey], [jkey, sskey], accum_out=ss)
    ts(k, "dve", ss, ss, 1.0 / n, EPS, ALU.mult, ALU.add, [sskey], [sskey])
    act(k, ss, ss, AF.Sqrt, [sskey], [sskey])
    k.op("dve", lambda e: e.reciprocal(out=ss, in_=ss), [sskey], [sskey])


def bcast_load(k, cx, vec_ap, n, key, q="sp"):
    t = cx.sb([128, n], F32, "bc")
    k.dma(q, t[:], vec_ap.partition_broadcast(128), writes=[key])
    return t


def t_to_f(k, cx, C, src, skeys, dst_dram, dkey, nch=2):
    stage = cx.sb([128, nch, L], BF16, "tfst")
    pr = cx.rot_ps(2, [128, nch, 128], BF16, "tfp")
    for i in range(NT):
        pt, pk = pr.next()
        for c in range(nch):
            tp(k, pt[:, c, :], src[:, i, c * 128:(c + 1) * 128], C["identb"][:], [skeys(i), "identb"], [pk], inc=(c == nch - 1))
        cp(k, "act" if i % 2 else "dve", stage[:, :, i * 128:(i + 1) * 128], pt[:], [pk], ["tfst%d" % i])
    k.dma("sp", dst_dram.rearrange("(c p) t -> p c t", p=128), stage[:], reads=["tfst%d" % i for i in range(NT)], writes=[dkey])


def phase_norm_T(nc, k, C, tag, Xsrc, xkey, g_ap, hT, hkey):
    with Ctx(nc, k, tag) as cx:
        gt = bcast_load(k, cx, g_ap, D, "g")
        xr = cx.rot_sb(2, [128, D], F32, "x")
        hr = cx.rot_sb(2, [128, D], BF16, "h")
        ssr = cx.rot_sb(2, [128, 1], F32, "ss")
        junk = cx.sb([128, D], F32, "junk")
        ptr = cx.rot_ps(2, [128, 8, 128], BF16, "pt")
        for i in range(NT):
            xt, xk = xr.next()
            k.dma("sp", xt[:], Xsrc[i * 128:(i + 1) * 128, :], reads=[xkey(i)], writes=[xk])
            ss, sk = ssr.next()
            rms_rstd(k, xt[:], xk, junk[:], "junk", ss[:], sk, D)
            h, hk = hr.next()
            stt(k, h[:], xt[:], ss[:, 0:1], gt[:], ALU.mult, ALU.mult, [xk, sk, "g"], [hk])
            pt, pk = ptr.next()
            for c in range(8):
                tp(k, pt[:, c, :], h[:, c * 128:(c + 1) * 128], C["identb"][:], [hk, "identb"], [pk], inc=(c == 7))
            cp(k, "act" if i % 2 else "dve", hT[:, :, i * 128:(i + 1) * 128], pt[:], [pk], [hkey(i)])


def phase_mla(nc, k, C, I, S, l, hT):
    sc = 96.0 ** -0.5
    with Ctx(nc, k, "mla%d" % l) as cx:
        wm = cx.sb([128, 8, 416], BF16, "wm")
        k.dma("pool", wm[:], I["w_in"][l, :, O_CQ:O_CQ + 416].rearrange("(c p) n -> p c n", p=128), writes=["wm"])
        wuq = cx.sb([128, 2, 384], BF16, "wuq")
        k.dma("pool", wuq[:], I["w_uq"][l].rearrange("(c p) n -> p c n", p=128), writes=["wuq"])
        wukv = cx.sb([128, 512], BF16, "wukv")
        k.dma("pool", wukv[:], I["w_ukv"][l], writes=["wukv"])
        gq = bcast_load(k, cx, I["q_norm_g"][l], 256, "gq")
        gkv = bcast_load(k, cx, I["kv_norm_g"][l], 128, "gkv")
        cs2 = cx.sb([128, NT, 32], F32, "cs2"); sn2 = cx.sb([128, NT, 32], F32, "sn2")
        cs2q = cx.sb([128, NT, 4, 32], F32, "cs2q"); sn2q = cx.sb([128, NT, 4, 32], F32, "sn2q")
        k.dma("sp", cs2[:], I["rope_cs2"], writes=["cs2"])
        k.dma("sp", sn2[:], I["rope_sn2"], writes=["sn2"])
        k.dma("sp", cs2q[:], I["rope_cs2q"], writes=["cs2q"])
        k.dma("sp", sn2q[:], I["rope_sn2q"], writes=["sn2q"])
        cqnT = cx.sb([128, 2, L], BF16, "cqnT"); ckvnT = cx.sb([128, L], BF16, "ckvnT")
        qT = cx.sb([128, 4, L], BF16, "qT"); kT = cx.sb([128, 4, L], BF16, "kT")
        vaug = cx.sb([128, NT, 4, 65], BF16, "vaug")
        ymla = cx.sb([128, NT, 256], BF16, "ymla")
        k.op("pool", lambda e: e.memset(vaug[:], 1.0), [], ["vaug_init"])
        qar = cx.rot_sb(3, [128, 4, 128], BF16, "qa"); kar = cx.rot_sb(3, [128, 4, 128], BF16, "ka")
        for j_, t_ in enumerate(qar.tiles):
            k.op("pool", lambda e: e.memset(t_[:], 0.0), [], ["mla%dqa#%d" % (l, j_)])
        for j_, t_ in enumerate(kar.tiles):
            k.op("pool", lambda e: e.memset(t_[:], 0.0), [], ["mla%dka#%d" % (l, j_)])
        with Ctx(nc, k, "mlaP%d" % l) as c2:
            pmr = c2.rot_ps(2, [128, 416], F32, "pm")
            ptr = c2.rot_ps(3, [128, 4, 128], BF16, "ptb")
            pqr = c2.rot_ps(1, [128, 384], F32, "pq")
            pkvr = c2.rot_ps(1, [128, 512], F32, "pkv")
            nrr = c2.rot_sb(4, [128, 416], BF16, "nrm")
            ssr = c2.rot_sb(8, [128, 1], F32, "ss")
            junk = c2.sb([128, 256], F32, "junk")
            tmr = c2.rot_sb(8, [128, 4, 32], F32, "tm")
            st = {}

            def stA(i):
                tsl = slice(i * 128, (i + 1) * 128)
                pm, pmk = pmr.next()
                for c in range(8):
                    mm(k, pm[:], hT[:, c, tsl], wm[:, c, :], c == 0, c == 7, ["hT%d" % i, "wm"], [pmk], inc=(c == 7))
                sq, sqk = ssr.next(); skv, skvk = ssr.next()
                rms_rstd(k, pm[:, 0:256], pmk, junk[:, 0:256], "junk", sq[:], sqk, 256)
                rms_rstd(k, pm[:, 256:384], pmk, junk[:, 0:128], "junk", skv[:], skvk, 128)
                nrm, nk = nrr.next()
                stt(k, nrm[:, 0:256], pm[:, 0:256], sq[:, 0:1], gq[:], ALU.mult, ALU.mult, [pmk, sqk, "gq"], [nk])
                stt(k, nrm[:, 256:384], pm[:, 256:384], skv[:, 0:1], gkv[:], ALU.mult, ALU.mult, [pmk, skvk, "gkv"], [nk])
                t1, t1k = tmr.next(); t2, t2k = tmr.next()
                tt(k, "dve", t1[:, 0, :], pm[:, 384:416], cs2[:, i, :], ALU.mult, [pmk, "cs2"], [t1k])
                tt(k, "dve", t2[:, 0, 0:16], pm[:, 400:416], sn2[:, i, 0:16], ALU.mult, [pmk, "sn2"], [t2k])
                tt(k, "dve", t2[:, 0, 16:32], pm[:, 384:400], sn2[:, i, 16:32], ALU.mult, [pmk, "sn2"], [t2k])
                tt(k, "dve", nrm[:, 384:416], t1[:, 0, :], t2[:, 0, :], ALU.add, [t1k, t2k], [nk])
                st[i] = {"nrm": nrm, "nk": nk}

            def stB(i):
                tsl = slice(i * 128, (i + 1) * 128)
                nrm, nk = st[i]["nrm"], st[i]["nk"]
                pt, ptk = ptr.next()
                for c in range(3):
                    tp(k, pt[:, c, :], nrm[:, c * 128:(c + 1) * 128], C["identb"][:], [nk, "identb"], [ptk], inc=(c == 2))
                cp(k, "act", cqnT[:, :, tsl], pt[:, 0:2, :], [ptk], ["cqnT%d" % i])
                cp(k, "act", ckvnT[:, tsl], pt[:, 2, :], [ptk], ["ckvnT%d" % i])

            def stC(i):
                tsl = slice(i * 128, (i + 1) * 128)
                nrm, nk = st[i]["nrm"], st[i]["nk"]
                qa, qak = qar.next(); ka, kak = kar.next()
                pq, pqk = pqr.next()
                for c in range(2):
                    mm(k, pq[:], cqnT[:, c, tsl], wuq[:, c, :], c == 0, c == 1, ["cqnT%d" % i, "wuq"], [pqk], inc=(c == 1))
                pqv = pq.rearrange("p (h d) -> p h d", h=4)
                ts(k, "dve", qa[:, :, 0:64], pqv[:, :, 0:64], sc, None, ALU.mult, None, [pqk], [qak])
                t3, t3k = tmr.next(); t4, t4k = tmr.next()
                tt(k, "dve", t3[:], pqv[:, :, 64:96], cs2q[:, i], ALU.mult, [pqk, "cs2q"], [t3k])
                tt(k, "dve", t4[:, :, 0:16], pqv[:, :, 80:96], sn2q[:, i, :, 0:16], ALU.mult, [pqk, "sn2q"], [t4k])
                tt(k, "dve", t4[:, :, 16:32], pqv[:, :, 64:80], sn2q[:, i, :, 16:32], ALU.mult, [pqk, "sn2q"], [t4k])
                tt(k, "dve", qa[:, :, 64:96], t3[:], t4[:], ALU.add, [t3k, t4k], [qak])
                pkv, pkvk = pkvr.next()
                mm(k, pkv[:], ckvnT[:, tsl], wukv[:], True, True, ["ckvnT%d" % i, "wukv"], [pkvk])
                pkvv = pkv.rearrange("p (h d) -> p h d", h=4)
                cp(k, "act", ka[:, :, 0:64], pkvv[:, :, 0:64], [pkvk], [kak])
                for h in range(4):
                    cp(k, "dve", ka[:, h, 64:96], nrm[:, 384:416], [nk], [kak])
                cp(k, "act", vaug[:, i, :, 0:64], pkvv[:, :, 64:128], [pkvk, "vaug_init"], ["vaug%d" % i])
                st[i].update({"qa": qa, "qak": qak, "ka": ka, "kak": kak})

            def stD(i):
                tsl = slice(i * 128, (i + 1) * 128)
                qa, qak, ka, kak = st[i]["qa"], st[i]["qak"], st[i]["ka"], st[i]["kak"]
                pt2, pt2k = ptr.next()
                for h in range(4):
                    tp(k, pt2[:, h, :], qa[:, h, :], C["identb"][:], [qak, "identb"], [pt2k], inc=(h == 3))
                cp(k, "dve", qT[:, :, tsl], pt2[:, :, :], [pt2k], ["qT%d" % i])
                pt3, pt3k = ptr.next()
                for h in range(4):
                    tp(k, pt3[:, h, :], ka[:, h, :], C["identb"][:], [kak, "identb"], [pt3k], inc=(h == 3))
                cp(k, "act", kT[:, :, tsl], pt3[:, :, :], [pt3k], ["kT%d" % i])
                del st[i]

            for step in range(NT + 3):
                if step < NT:
                    stA(step)
                if 0 <= step - 1 < NT:
                    stB(step - 1)
                if 0 <= step - 2 < NT:
                    stC(step - 2)
                if 0 <= step - 3 < NT:
                    stD(step - 3)
        with Ctx(nc, k, "mlaA%d" % l) as c3:
            psr = c3.rot_ps(4, [128, 512], F32, "s")
            por = c3.rot_ps(2, [128, 4, 128], F32, "o")
            ppr = c3.rot_sb(4, [128, 512], BF16, "pT")
            rcr = c3.rot_sb(2, [128, 4, 1], F32, "rc")
            allq = ["qT%d" % i for i in range(NT)]
            items = [(h, qb, kt) for h in range(4) for qb in range(4) for kt in range(NT)]
            LOOK = 2
            sbuf = {}

            def emit_S(idx):
                h, qb, kt = items[idx]
                ps_, psk = psr.next()
                mm(k, ps_[:], kT[:, h, kt * 128:(kt + 1) * 128], qT[:, h, qb * 512:(qb + 1) * 512], True, True,
                   ["kT%d" % kt] + allq[qb * 4:qb * 4 + 4], [psk])
                sbuf[idx] = (ps_, psk)

            for j in range(min(LOOK, len(items))):
                emit_S(j)
            po = pok = None
            for idx, (h, qb, kt) in enumerate(items):
                if idx + LOOK < len(items):
                    emit_S(idx + LOOK)
                if kt == 0:
                    po, pok = por.next()
                ps_, psk = sbuf.pop(idx)
                pT, pTk = ppr.next()
                act(k, pT[:], ps_[:], AF.Exp, [psk], [pTk])
                for qs in range(4):
                    mm(k, po[:, qs, 0:65], pT[:, qs * 128:(qs + 1) * 128], vaug[:, kt, h, :], kt == 0 and qs == 0, kt == NT - 1 and qs == 3,
                       [pTk, "vaug%d" % kt], [pok], inc=(qs == 3))
                if kt == NT - 1:
                    rc, rck = rcr.next()
                    k.op("dve", lambda e: e.reciprocal(out=rc[:], in_=po[:, :, 64:65]), [pok], [rck])
                    tt(k, "dve", ymla[:, qb * 4:(qb + 1) * 4, h * 64:(h + 1) * 64], po[:, :, 0:64],
                       rc[:].broadcast_to([128, 4, 64]), ALU.mult, [pok, rck], ["ymla%d" % qb])
        with Ctx(nc, k, "mlaT%d" % l) as c4:
            t_to_f(k, c4, C, ymla, lambda i: "ymla%d" % (i // 4), S["Yd"][2], "Yd2")


def phase_na(nc, k, C, I, S, l, hT):
    sc = 64.0 ** -0.5
    with Ctx(nc, k, "na%d" % l) as cx:
        wna = cx.sb([128, 8, 768], BF16, "wna")
        k.dma("pool", wna[:], I["w_in"][l, :, O_NA:O_NA + 768].rearrange("(c p) n -> p c n", p=128), writes=["wna"])
        qT = cx.sb([128, 2, L], BF16, "qT"); kT = cx.sb([128, 2, L], BF16, "kT")
        va0 = cx.sb([128, NT, 4, 65], BF16, "va0"); va1 = cx.sb([128, NT, 4, 65], BF16, "va1")
        yna = cx.sb([128, NT, 256], BF16, "yna")
        T2 = cx.sb([128, 4, 14, 64], BF16, "T2")
        k.op("pool", lambda e: e.memset(va0[:], 1.0), [], ["va0i"])
        k.op("pool", lambda e: e.memset(va1[:], 1.0), [], ["va1i"])
        with Ctx(nc, k, "naB%d" % l) as cb:
            rp = cb.sb([32, 60], F32, "rp")
            k.op("pool", lambda e: e.memset(rp[:], 1.0), [], ["rp"])
            k.dma("sp", rp[0:31, :], I["rpb"][l].rearrange("h r c -> c (h r)"), reads=["rp"], writes=["rp"], allow_slow_non_contiguous=True)
            E = cb.sb([32, 4096], F32, "E")
            k.dma("sp", E[:], I["na_E"], writes=["E"])
            bsb = cb.sb([60, 4096], F32, "bsb")
            pbr = cb.rot_ps(2, [128, 512], F32, "pb")
            for j in range(8):
                pb, pbk = pbr.next()
                mm(k, pb[0:60, :], rp[:, :], E[:, j * 512:(j + 1) * 512], True, True, ["rp", "E"], [pbk])
                cp(k, "act" if j % 2 else "dve", bsb[:, j * 512:(j + 1) * 512], pb[0:60, :], [pbk], ["bsb"])
            k.dma("sp", S["bias_d"], bsb[:], reads=["bsb"], writes=["bias_d"])
            T2f = cb.sb([128, 4, 14, 64], F32, "T2f")
            bv = S["bias_d"].rearrange("(h r) (kc qc) -> kc h r qc", h=4, kc=64)
            for h in range(4):
                k.dma("sp", T2f[0:64, h], bv[:, h, 0:14, :], reads=["bias_d"], writes=["T2f"])
                k.dma("sp", T2f[64:128, h], bv[:, h, 1:15, :], reads=["bias_d"], writes=["T2f"])
            cp(k, "dve", T2[:], T2f[:], ["T2f"], ["T2"])
        with Ctx(nc, k, "naP%d" % l) as c2:
            psr = c2.rot_ps(3, [128, 512], F32, "ps")
            for cc in range(4):
                for tb in range(4):
                    ps_, psk = psr.next()
                    hk = ["hT%d" % (tb * 4 + j) for j in range(4)]
                    for c in range(8):
                        mm(k, ps_[:], wna[:, c, cc * 128:(cc + 1) * 128], hT[:, c, tb * 512:(tb + 1) * 512], c == 0, c == 7, hk + ["wna"], [psk], inc=(c == 7))
                    if cc < 2:
                        k.op("act", lambda e: e.mul(qT[:, cc, tb * 512:(tb + 1) * 512], ps_[:], sc), [psk], ["qT%d_%d" % (cc, tb)])
                    else:
                        cp(k, "dve", kT[:, cc - 2, tb * 512:(tb + 1) * 512], ps_[:], [psk], ["kT%d_%d" % (cc - 2, tb)])
            for i in range(NT):
                ps_, psk = psr.next()
                for c in range(8):
                    mm(k, ps_[:, 0:256], hT[:, c, i * 128:(i + 1) * 128], wna[:, c, 512:768], c == 0, c == 7, ["hT%d" % i, "wna"], [psk], inc=(c == 7))
                cp(k, "act" if i % 2 else "dve", va0[:, i, :, 0:64], ps_[:, 0:256].rearrange("p (h d) -> p h d", h=4), [psk, "va0i"], ["va0_%d" % i])
            for i in range(NT - 1):
                ps_, psk = psr.next()
                for c in range(8):
                    mm(k, ps_[:, 0:256], hT[:, c, 64 + i * 128:64 + (i + 1) * 128], wna[:, c, 512:768], c == 0, c == 7, ["hT%d" % i, "hT%d" % (i + 1), "wna"], [psk], inc=(c == 7))
                cp(k, "act" if i % 2 else "dve", va1[:, i, :, 0:64], ps_[:, 0:256].rearrange("p (h d) -> p h d", h=4), [psk, "va1i"], ["va1_%d" % i])
        with Ctx(nc, k, "naA%d" % l) as c3:
            psr = c3.rot_ps(4, [128, 4, 64], F32, "s")
            por = c3.rot_ps(2, [128, 4, 128], F32, "o")
            ppr = c3.rot_sb(4, [128, 4, 64], BF16, "pT")
            rcr = c3.rot_sb(2, [128, 4, 1], F32, "rc")
            items = [(r, h) for r in range(32) for h in range(4)]
            LOOK = 2
            sbuf = {}

            def emit_S(idx):
                r, h = items[idx]
                r0 = min(max(r - 4, 0), 24)
                ch, hp = h // 2, (h % 2) * 64
                ps_, psk = psr.next()
                for j in range(4):
                    ktok = (r0 + 2 * j) * 64
                    dr1 = r0 + 2 * j - r + 7
                    kkeys = ["kT%d_%d" % (ch, tbb) for tbb in sorted(set([ktok // 512, (ktok + 127) // 512]))]
                    mm(k, ps_[:, j, :], kT[hp:hp + 64, ch, ktok:ktok + 128], qT[hp:hp + 64, ch, r * 64:(r + 1) * 64], True, False,
                       kkeys + ["qT%d_%d" % (ch, r // 8)], [psk], inc=False)
                    mm(k, ps_[:, j, :], C["identb"][:], T2[:, h, dr1, :], False, True, ["identb", "T2"], [psk], inc=(j == 3))
                sbuf[idx] = (ps_, psk)

            for j in range(LOOK):
                emit_S(j)
            po = pok = None
            for idx, (r, h) in enumerate(items):
                if idx + LOOK < len(items):
                    emit_S(idx + LOOK)
                r0 = min(max(r - 4, 0), 24)
                if r % 2 == 0 and h == 0:
                    po, pok = por.next()
                ro = (r % 2) * 64
                ps_, psk = sbuf.pop(idx)
                pT, pTk = ppr.next()
                act(k, pT[:], ps_[:], AF.Exp, [psk], [pTk])
                for j in range(4):
                    rr = r0 + 2 * j
                    if rr % 2 == 0:
                        vs, vkey = va0[:, rr // 2, h, :], "va0_%d" % (rr // 2)
                    else:
                        vs, vkey = va1[:, (rr - 1) // 2, h, :], "va1_%d" % ((rr - 1) // 2)
                    mm(k, po[ro:ro + 64, h, 0:65], pT[:, j, :], vs, j == 0, j == 3, [pTk, vkey], [pok], inc=(j == 3))
                if r % 2 == 1 and h == 3:
                    ti = r // 2
                    rc, rck = rcr.next()
                    k.op("dve", lambda e: e.reciprocal(out=rc[:], in_=po[:, :, 64:65]), [pok], [rck])
                    tt(k, "dve", yna[:, ti, :].rearrange("p (h d) -> p h d", h=4), po[:, :, 0:64],
                       rc[:].broadcast_to([128, 4, 64]), ALU.mult, [pok, rck], ["yna%d" % ti])
        with Ctx(nc, k, "naT%d" % l) as c4:
            t_to_f(k, c4, C, yna, lambda i: "yna%d" % i, S["Yd"][3], "Yd3")


def phase_hyfn_prep(nc, k, C, I, S, l, hT):
    import os as _os
    PLV = int(_os.environ.get("PREP_LV", "9"))
    with Ctx(nc, k, "hp%d" % l) as cx:
        wh = cx.sb([128, 8, 1024], BF16, "wh")
        for j in range(2):
            k.dma("pool", wh[:, :, j * 512:(j + 1) * 512], I["w_in"][l, :, j * 512:(j + 1) * 512].rearrange("(c p) n -> p c n", p=128), writes=["wh%d" % j])
        whk = ["wh0", "wh1"]
        cw = cx.sb([128, 6, 3], F32, "cw")
        for kk_ in range(3):
            k.dma("sp", cw[:, :, kk_], I["hy_conv_w"][l, kk_].rearrange("(cc p) -> p cc", p=128), writes=["cw"], allow_slow_non_contiguous=True)
        cbias = cx.sb([128, 6], F32, "cb")
        k.dma("sp", cbias[:], I["hy_conv_b"][l].rearrange("(cc p) -> p cc", p=128), writes=["cb"], allow_slow_non_contiguous=True)
        upr = cx.rot_sb(2, [128, L + 2], F32, "up")
        for t_, kk_ in zip(upr.tiles, ["hp%dup#0" % l, "hp%dup#1" % l]):
            k.op("pool", lambda e: e.memset(t_[:, 0:1], 0.0), [], [kk_])
            k.op("pool", lambda e: e.memset(t_[:, L + 1:L + 2], 0.0), [], [kk_])
        ucr = cx.rot_sb(2, [128, L], F32, "uc")
        vbr = cx.rot_sb(2, [128, L], BF16, "vb")
        vst = cx.sb([128, NT, 256], BF16, "vst")
        psr = cx.rot_ps(3, [128, 512], F32, "ps")
        ptr = cx.rot_ps(2, [128, 4, 128], BF16, "pt")
        dsts = [S["x1Td"], S["x2Td"], S["vTd"]]
        for cc in range(6):
            up, upk = upr.next()
            for tb in range(4):
                ps_, psk = psr.next()
                hk = ["hT%d" % (tb * 4 + j) for j in range(4)]
                for c in range(8):
                    mm(k, ps_[:], wh[:, c, cc * 128:(cc + 1) * 128], hT[:, c, tb * 512:(tb + 1) * 512], c == 0, c == 7, hk + whk, [psk], inc=(c == 7))
                cp(k, "act" if tb % 2 else "dve", up[:, 1 + tb * 512:1 + (tb + 1) * 512], ps_[:], [psk], [upk])
            if PLV <= 1:
                continue
            u_c, uck = ucr.next()
            ts(k, "dve", u_c[:], up[:, 1:L + 1], cw[:, cc, 1:2], cbias[:, cc:cc + 1], ALU.mult, ALU.add, [upk, "cw", "cb"], [uck])
            stt(k, u_c[:], up[:, 0:L], cw[:, cc, 0:1], u_c[:], ALU.mult, ALU.add, [upk, "cw", uck], [uck])
            stt(k, u_c[:], up[:, 2:L + 2], cw[:, cc, 2:3], u_c[:], ALU.mult, ALU.add, [upk, "cw", uck], [uck])
            dst = dsts[cc // 2][(cc % 2) * 128:(cc % 2) * 128 + 128, :]
            k.dma("sp", dst, u_c[:], reads=[uck], writes=["hyd%d" % cc])
            if PLV <= 2:
                continue
            if cc >= 4:
                vb, vbk = vbr.next()
                cp(k, "pool", vb[:], u_c[:], [uck], [vbk])
                for g in range(4):
                    pt, ptk = ptr.next()
                    for j in range(4):
                        i = g * 4 + j
                        tp(k, pt[:, j, :], vb[:, i * 128:(i + 1) * 128], C["identb"][:], [vbk, "identb"], [ptk], inc=(j == 3))
                    cp(k, "act" if g % 2 else "dve", vst[:, g * 4:(g + 1) * 4, (cc - 4) * 128:(cc - 3) * 128], pt[:], [ptk], ["vst%d" % (cc - 4)])
        if PLV <= 3:
            return
        k.dma("sp", S["v_d"].rearrange("(c p) n -> p c n", p=128), vst[:], reads=["vst0", "vst1"], writes=["v_d"])
        if PLV <= 4:
            return
        PX = _os.environ.get("PREPX", "")
        sgn = cx.sb([128, 1], F32, "sgn")
        if "nodma" in PX:
            k.op("dve", lambda e: e.memset(sgn[:], 1.0), [], ["sgn"])
        else:
            k.dma("sp", sgn[:], I["signp"], writes=["sgn"])
        uf0 = cx.sb([128, NT, 256], BF16, "uf0"); uf1 = cx.sb([128, NT, 256], BF16, "uf1")
        for i in range(8 if "half" in PX else NT):
            ps_, psk = psr.next()
            for c in range(8):
                mm(k, ps_[:, 0:256], hT[:, c, i * 128:(i + 1) * 128], wh[:, c, 768:1024], c == 0, c == 7, ["hT%d" % i] + whk, [psk], inc=(c == 7))
            if "noact" not in PX:
                cp(k, "act", uf0[:, i, :], ps_[:, 0:256], [psk], ["uf0"])
            if "nodve" not in PX:
                ts(k, "dve", uf1[:, i, :], ps_[:, 0:256], sgn[:, 0:1], None, ALU.mult, None, [psk, "sgn"], ["uf1"])
        if "nodma" in PX:
            return
        k.dma("sp", S["fn_d"][0].rearrange("(c p) n -> p c n", p=128), uf0[:], reads=["uf0"], writes=["fn_d0"])
        k.dma("sp", S["fn_d"][1].rearrange("(c p) n -> p c n", p=128), uf1[:], reads=["uf1"], writes=["fn_d1"])


def phase_dft(nc, k, C, I, S, l):
    TWO_PI = 2.0 * math.pi
    import os as _os
    HLV = int(_os.environ.get("HY_LV", "9"))
    if HLV <= 1:
        return
    with Ctx(nc, k, "dft%d" % l) as cx:
        A = cx.sb([128, NT, L], BF16, "A"); B = cx.sb([128, NT, L], BF16, "B")
        Av = I["dftA"].rearrange("(c p) f -> p c f", p=128); Bv = I["dftB"].rearrange("(c p) f -> p c f", p=128)
        for j in range(4):
            k.dma("sp", A[:, j * 4:(j + 1) * 4, :], Av[:, j * 4:(j + 1) * 4, :], writes=["A%d" % j])
            k.dma("act", B[:, j * 4:(j + 1) * 4, :], Bv[:, j * 4:(j + 1) * 4, :], writes=["B%d" % j])
        AK = ["A%d" % j for j in range(4)]; BK = ["B%d" % j for j in range(4)]
        altcol = cx.sb([128, 1], BF16, "altcol"); altrow = cx.sb([1, L], BF16, "altrow")
        k.dma("pool", altcol[:], I["altcol"], writes=["altcol"])
        k.dma("pool", altrow[:], I["altrow"], writes=["altrow"])
        sk = cx.sb([128, 2, 2], F32, "skip")
        for o_ in range(2):
            k.dma("sp", sk[:, o_, :], I["hy_skip"][l, o_].rearrange("(cc p) -> p cc", p=128), writes=["skip"], allow_slow_non_contiguous=True)
        if HLV <= 2:
            return
        with Ctx(nc, k, "flt%d" % l) as cf:
            hf2T = cf.sb([64, L], F32, "hf2T")
            w3 = cf.sb([64, 1024], F32, "w3")
            k.dma("sp", w3[:], I["hf_w3"][l], writes=["w3"])
            onesf = cf.sb([128, 128], F32, "onesf")
            k.dma("sp", onesf[:], I["ones_f"], writes=["onesf"])
            with Ctx(nc, k, "mlp%d" % l) as cm:
                zT = cm.sb([33, L], F32, "zT")
                k.dma("sp", zT[:], I["hy_zT"], writes=["zT"])
                w1 = cm.sb([33, 64], F32, "w1"); w2 = cm.sb([64, 64], F32, "w2")
                k.dma("sp", w1[:], I["hf_w1"][l], writes=["w1"])
                k.dma("sp", w2[:], I["hf_w2"][l], writes=["w2"])
                bb = cm.sb([64, 2], F32, "bb"); fr = cm.sb([64, 2], F32, "fr")
                k.dma("sp", bb[:, 0:1], I["hf_b1"][l].rearrange("(p o) -> p o", o=1), writes=["bb"])
                k.dma("sp", bb[:, 1:2], I["hf_b2"][l].rearrange("(p o) -> p o", o=1), writes=["bb"])
                k.dma("sp", fr[:], I["hf_freq"][l].rearrange("k p -> p k"), writes=["fr"], allow_slow_non_contiguous=True)
                fb = cm.sb([64, 2], F32, "fb")
                tt(k, "dve", fb[:], fr[:], bb[:], ALU.mult, ["fr", "bb"], ["fb"])
                negpi = cm.sb([64, 1], F32, "negpi")
                k.op("pool", lambda e: e.memset(negpi[:], -math.pi), [], ["negpi"])
                hf1T = cm.sb([64, L], F32, "hf1T"); arg = cm.sb([64, L], F32, "arg"); nn = cm.sb([64, L], F32, "nn")
                pmr = cm.rot_ps(2, [128, 512], F32, "pm")
                for (W, src, srck, dst, dstk, j, kdim) in ((w1, zT, "zT", hf1T, "hf1T", 0, 33), (w2, hf1T, "hf1T", hf2T, "hf2T", 1, 64)):
                    for tb in range(4):
                        tsl = slice(tb * 512, (tb + 1) * 512)
                        pm, pmk = pmr.next()
                        mm(k, pm[0:64, :], W[0:kdim, :], src[0:kdim, tsl], True, True, ["w1", "w2", "zT" if j == 0 else "hf1T%d" % tb], [pmk])
                        ts(k, "dve", arg[:, tsl], pm[0:64, :], fr[:, j:j + 1], fb[:, j:j + 1], ALU.mult, ALU.add, [pmk, "fr", "fb"], ["arg%d" % tb])
                        MAGIC = 12582912.0
                        ts(k, "dve", nn[:, tsl], arg[:, tsl], 1.0 / TWO_PI, MAGIC, ALU.mult, ALU.add, ["arg%d" % tb], ["nn%d" % tb])
                        ts(k, "dve", nn[:, tsl], nn[:, tsl], -MAGIC, None, ALU.add, None, ["nn%d" % tb], ["nn%d" % tb])
                        stt(k, arg[:, tsl], nn[:, tsl], -TWO_PI, arg[:, tsl], ALU.mult, ALU.add, ["nn%d" % tb, "arg%d" % tb], ["arg%d" % tb])
                        ts(k, "dve", arg[:, tsl], arg[:, tsl], -math.pi, math.pi, ALU.max, ALU.min, ["arg%d" % tb], ["arg%d" % tb])
                        act(k, dst[:, tsl], arg[:, tsl], AF.Sin, ["arg%d" % tb], [dstk + str(tb)])
            if HLV <= 3:
                return
            kkr = cf.rot_sb(3, [128, 512], F32, "kk")
            P = cf.sb([128, NT, 256], BF16, "P"); Q = cf.sb([128, NT, 256], BF16, "Q")
            wtr = cf.rot_sb(2, [128, 256], F32, "wt")
            sqr = cf.rot_sb(2, [128, 512], F32, "sq")
            tmr = cf.rot_sb(2, [128, 256], F32, "tm")
            kor = cf.rot_sb(4, [128, 256], F32, "ko")
            pss_sb = cf.sb([128, 512], F32, "pss_sb")
            rn = cf.sb([128, 256], F32, "rn")
            kn = cf.sb([1, 256], F32, "kn")
            p3r = cf.rot_ps(2, [128, 512], F32, "p3")
            pssr = cf.rot_ps(1, [128, 512], F32, "pss")
            par = cf.rot_ps(2, [128, 256], F32, "pa")
            pbr = cf.rot_ps(2, [128, 256], F32, "pb")
            pnr = cf.rot_ps(1, [128, 256], F32, "pn")
            hfk = ["hf2T%d" % tb for tb in range(4)]

            def kk_tile(o, i):
                p3, p3k = p3r.next()
                mm(k, p3[:], hf2T[:, i * 128:(i + 1) * 128], w3[:, o * 512:(o + 1) * 512], True, True, [hfk[i // 4], "w3"], [p3k])
                wt, wtk = wtr.next()
                k.dma("sp", wt[:], I["hy_win"][i * 128:(i + 1) * 128, :], writes=[wtk])
                kt_, ktk = kkr.next()
                tt(k, "dve", kt_[:].rearrange("p (a c) -> p a c", a=2), p3[:].rearrange("p (a c) -> p a c", a=2),
                   wt[:].unsqueeze(1).broadcast_to([128, 2, 256]), ALU.mult, [p3k, wtk], [ktk])
                if i == 0:
                    k.op("pool", lambda e: e.memset(kt_[0:1, 256:512], 0.0), [ktk], [ktk])
                return kt_, ktk

            for o in range(2):
                pss, pssk = pssr.next()
                for i in range(NT):
                    kt_, ktk = kk_tile(o, i)
                    sq, sqk = sqr.next()
                    act(k, sq[:], kt_[:], AF.Square, [ktk], [sqk])
                    mm(k, pss[:], onesf[:], sq[:], i == 0, i == NT - 1, ["onesf", sqk], [pssk], inc=True)
                cp(k, "act", pss_sb[:], pss[:], [pssk], ["pss_sb"])
                tt(k, "dve", rn[:], pss_sb[:, 0:256], pss_sb[:, 256:512], ALU.add, ["pss_sb"], ["rn"])
                ts(k, "dve", rn[:], rn[:], EPS, None, ALU.add, None, ["rn"], ["rn"])
                act(k, rn[:], rn[:], AF.Sqrt, ["rn"], ["rn"])
                k.op("dve", lambda e: e.reciprocal(out=rn[:], in_=rn[:]), ["rn"], ["rn"])
                for i in range(NT):
                    kt_, ktk = kk_tile(o, i)
                    tm, tmk = tmr.next()
                    tt(k, "pool", tm[:], kt_[:, 0:256], kt_[:, 256:512], ALU.add, [ktk], [tmk])
                    tt(k, "dve", P[:, i, :], tm[:], rn[:], ALU.mult, [tmk, "rn"], ["P%d" % i])
                    tm2, tm2k = tmr.next()
                    tt(k, "pool", tm2[:], kt_[:, 0:256], kt_[:, 256:512], ALU.subtract, [ktk], [tm2k])
                    tt(k, "dve", Q[:, i, :], tm2[:], rn[:], ALU.mult, [tm2k, "rn"], ["Q%d" % i])
                PK = ["P%d" % i for i in range(NT)]; QK = ["Q%d" % i for i in range(NT)]
                for fc in range(NT):
                    pa, pak = par.next(); pb, pbk = pbr.next()
                    for tc in range(NT):
                        mm(k, pa[:], A[:, tc, fc * 128:(fc + 1) * 128], P[:, tc, :], tc == 0, tc == NT - 1, [AK[tc // 4], PK[tc]], [pak], inc=(tc == NT - 1))
                    for tc in range(NT):
                        mm(k, pb[:], B[:, tc, fc * 128:(fc + 1) * 128], Q[:, tc, :], tc == 0, tc == NT - 1, [BK[tc // 4], QK[tc]], [pbk], inc=(tc == NT - 1))
                    ka, kak = kor.next(); kb, kbk = kor.next()
                    act(k, ka[:], pa[:], AF.Copy, [pak], [kak], scale=2.0 / NFFT)
                    ts(k, "dve", kb[:], pb[:], 2.0 / NFFT, None, ALU.mult, None, [pbk], [kbk])
                    if fc == 0:
                        ts(k, "dve", ka[0:1, :], ka[0:1, :], 0.5, None, ALU.mult, None, [kak], [kak])
                    k.dma("sp", S["KAd"][o, fc * 128:(fc + 1) * 128, :], ka[:], reads=[kak], writes=["KAd%d_%d" % (o, fc)])
                    k.dma("sp", S["KBd"][o, fc * 128:(fc + 1) * 128, :], kb[:], reads=[kbk], writes=["KBd%d_%d" % (o, fc)])
                pn, pnk = pnr.next()
                for tc in range(NT):
                    mm(k, pn[0:1, :], altcol[:, 0:1], P[:, tc, :], tc == 0, tc == NT - 1, ["altcol", PK[tc]], [pnk], inc=(tc == NT - 1))
                ts(k, "dve", kn[:], pn[0:1, :], 1.0 / NFFT, None, ALU.mult, None, [pnk], ["kn"])
                k.dma("sp", S["KNd"][o:o + 1, :], kn[:], reads=["kn"], writes=["KNd%d" % o])
        if HLV <= 4:
            return
        for o in range(2):
            if HLV <= 5 and o == 1:
                break
            with Ctx(nc, k, "cv%d_%d" % (l, o)) as cc:
                z = cc.sb([128, NT, 256], BF16, "z")
                k.dma("sp", z[:], (S["v_d"] if o == 0 else S["z2_d"]).rearrange("(c p) n -> p c n", p=128),
                      reads=["v_d" if o == 0 else "z2_d"], writes=["z"])
                YA = cc.sb([128, NT, 256], BF16, "YA"); YB = cc.sb([128, NT, 256], BF16, "YB")
                kn = cc.sb([1, 256], F32, "kn")
                k.dma("sp", kn[:], S["KNd"][o:o + 1, :], reads=["KNd%d" % o], writes=["kn"])
                yn = cc.sb([1, 256], BF16, "yn")
                with Ctx(nc, k, "cvf%d_%d" % (l, o)) as c1:
                    par = c1.rot_ps(2, [128, 256], F32, "pa"); pbr = c1.rot_ps(2, [128, 256], F32, "pb")
                    pnr = c1.rot_ps(1, [128, 256], F32, "pn")
                    kar = c1.rot_sb(3, [128, 256], F32, "ka"); kbr = c1.rot_sb(3, [128, 256], F32, "kb")
                    tmr = c1.rot_sb(8, [128, 256], F32, "tm")
                    for fc in range(NT):
                        pa, pak = par.next(); pb, pbk = pbr.next()
                        for tc in range(NT):
                            mm(k, pa[:], A[:, tc, fc * 128:(fc + 1) * 128], z[:, tc, :], tc == 0, tc == NT - 1, [AK[tc // 4], "z"], [pak], inc=(tc == NT - 1))
                        for tc in range(NT):
                            mm(k, pb[:], B[:, tc, fc * 128:(fc + 1) * 128], z[:, tc, :], tc == 0, tc == NT - 1, [BK[tc // 4], "z"], [pbk], inc=(tc == NT - 1))
                        ka, kak = kar.next(); kb, kbk = kbr.next()
                        k.dma("sp", ka[:], S["KAd"][o, fc * 128:(fc + 1) * 128, :], reads=["KAd%d_%d" % (o, fc)], writes=[kak])
                        k.dma("sp", kb[:], S["KBd"][o, fc * 128:(fc + 1) * 128, :], reads=["KBd%d_%d" % (o, fc)], writes=[kbk])
                        t1, t1k = tmr.next(); t2, t2k = tmr.next(); t3, t3k = tmr.next(); t4, t4k = tmr.next()
                        tt(k, "dve", t1[:], pa[:], ka[:], ALU.mult, [pak, kak], [t1k])
                        tt(k, "dve", t2[:], pb[:], kb[:], ALU.mult, [pbk, kbk], [t2k])
                        tt(k, "dve", t3[:], pa[:], kb[:], ALU.mult, [pak, kbk], [t3k])
                        tt(k, "dve", t4[:], pb[:], ka[:], ALU.mult, [pbk, kak], [t4k])
                        tt(k, "pool", YA[:, fc, :], t1[:], t2[:], ALU.subtract, [t1k, t2k], ["YA%d" % fc])
                        tt(k, "pool", YB[:, fc, :], t3[:], t4[:], ALU.add, [t3k, t4k], ["YB%d" % fc])
                    pn, pnk = pnr.next()
                    for tc in range(NT):
                        mm(k, pn[0:1, :], altcol[:, 0:1], z[:, tc, :], tc == 0, tc == NT - 1, ["altcol", "z"], [pnk], inc=(tc == NT - 1))
                    tt(k, "dve", yn[:], pn[0:1, :], kn[:], ALU.mult, [pnk, "kn"], ["yn"])
                with Ctx(nc, k, "cvi%d_%d" % (l, o)) as c2:
                    pyr = c2.rot_ps(2, [128, 512], F32, "py")
                    ptr = c2.rot_ps(2, [128, 4, 128], BF16, "pt")
                    xr = c2.rot_sb(2, [128, 512], F32, "xm"); sr = c2.rot_sb(2, [128, 512], F32, "sm")
                    tmr = c2.rot_sb(2, [128, 512], F32, "tm"); zr = c2.rot_sb(2, [128, 512], F32, "zo")
                    zbr = c2.rot_sb(2, [128, 512], BF16, "zb")
                    if o == 0:
                        z2st = c2.sb([128, NT, 256], BF16, "z2st")
                    else:
                        yst = c2.sb([128, 2, L], BF16, "yst")
                    YAK = ["YA%d" % fc for fc in range(NT)]; YBK = ["YB%d" % fc for fc in range(NT)]
                    xsrc = S["x1Td"] if o == 0 else S["x2Td"]
                    ssrc = S["vTd"] if o == 0 else S["z2Td"]
                    for cch in range(2):
                        csl = slice(cch * 128, (cch + 1) * 128)
                        for tb in range(4):
                            tsl = slice(tb * 512, (tb + 1) * 512)
                            py, pyk = pyr.next()
                            for fc in range(NT):
                                mm(k, py[:], YA[:, fc, csl], A[:, fc, tsl], fc == 0, False, [YAK[fc], AK[fc // 4]], [pyk], inc=False)
                            for fc in range(NT):
                                mm(k, py[:], YB[:, fc, csl], B[:, fc, tsl], False, False, [YBK[fc], BK[fc // 4]], [pyk], inc=False)
                            mm(k, py[:], yn[0:1, csl], altrow[0:1, tsl], False, True, ["yn", "altrow"], [pyk], inc=True)
                            xm, xmk = xr.next(); sm, smk = sr.next()
                            skeys = (["hyd%d" % (4 + cch)] if o == 0 else ["z2Td%d_%d" % (cch, tb)])
                            k.dma("sp", xm[:], xsrc[csl, tsl], reads=["hyd%d" % ((0 if o == 0 else 2) + cch)], writes=[xmk])
                            k.dma("sp", sm[:], ssrc[csl, tsl], reads=skeys, writes=[smk])
                            tm, tmk = tmr.next()
                            stt(k, tm[:], sm[:], sk[:, o, cch:cch + 1], py[:], ALU.mult, ALU.add, [smk, "skip", pyk], [tmk])
                            if o == 0:
                                zo, zok = zr.next()
                                tt(k, "dve", zo[:], tm[:], xm[:], ALU.mult, [tmk, xmk], [zok])
                                k.dma("sp", S["z2Td"][csl, tsl], zo[:], reads=[zok], writes=["z2Td%d_%d" % (cch, tb)])
                                zb, zbk = zbr.next()
                                cp(k, "act", zb[:], zo[:], [zok], [zbk])
                                pt, ptk = ptr.next()
                                for j in range(4):
                                    tp(k, pt[:, j, :], zb[:, j * 128:(j + 1) * 128], C["identb"][:], [zbk, "identb"], [ptk], inc=(j == 3))
                                cp(k, "act", z2st[:, tb * 4:(tb + 1) * 4, csl], pt[:], [ptk], ["z2st"])
                            else:
                                tt(k, "dve", yst[:, cch, tsl], tm[:], xm[:], ALU.mult, [tmk, xmk], ["yst"])
                    if o == 0:
                        k.dma("sp", S["z2_d"].rearrange("(c p) n -> p c n", p=128), z2st[:], reads=["z2st"], writes=["z2_d"])
                    else:
                        k.dma("sp", S["Yd"][0].rearrange("(c p) t -> p c t", p=128), yst[:], reads=["yst"], writes=["Yd0"])
        if HLV <= 6:
            return
        with Ctx(nc, k, "fn%d" % l) as cf:
            uf0 = cf.sb([128, NT, 256], BF16, "uf0"); uf1 = cf.sb([128, NT, 256], BF16, "uf1")
            k.dma("sp", uf0[:], S["fn_d"][0].rearrange("(c p) n -> p c n", p=128), reads=["fn_d0"], writes=["uf0"])
            k.dma("sp", uf1[:], S["fn_d"][1].rearrange("(c p) n -> p c n", p=128), reads=["fn_d1"], writes=["uf1"])
            fC = cf.sb([128, 2, 256], BF16, "fC"); fS = cf.sb([128, 2, 256], BF16, "fS")
            k.dma("pool", fC[:], I["fn_C"].rearrange("(c p) n -> p c n", p=128), writes=["fC"])
            k.dma("pool", fS[:], I["fn_S"].rearrange("(c p) n -> p c n", p=128), writes=["fS"])
            VT = cf.sb([128, 2, 2, L], BF16, "VT")
            yst = cf.sb([128, 2, L], BF16, "yst")
            pyr = cf.rot_ps(3, [128, 512], F32, "py")
            for cs, (M_, MK) in enumerate(((A, AK), (B, BK))):
                for cch in range(2):
                    for lb in range(4):
                        src, srck = (uf0, "uf0") if lb < 2 else (uf1, "uf1")
                        base = (lb % 2) * 1024
                        py, pyk = pyr.next()
                        for lc in range(NT):
                            mm(k, py[:], src[:, lc, cch * 128:(cch + 1) * 128], M_[:, lc, base:base + 1024:2], lc == 0, lc == NT - 1, [srck, MK[lc // 4]], [pyk], inc=(lc == NT - 1))
                        cp(k, "act" if lb % 2 else "dve", VT[:, cs, cch, lb * 512:(lb + 1) * 512], py[:], [pyk], ["VT%d_%d_%d" % (cs, cch, lb)])
            for cch in range(2):
                for lb in range(4):
                    py, pyk = pyr.next()
                    mm(k, py[:], fC[:, cch, cch * 128:(cch + 1) * 128], VT[:, 0, cch, lb * 512:(lb + 1) * 512], True, False, ["fC", "VT0_%d_%d" % (cch, lb)], [pyk], inc=False)
                    mm(k, py[:], fS[:, cch, cch * 128:(cch + 1) * 128], VT[:, 1, cch, lb * 512:(lb + 1) * 512], False, True, ["fS", "VT1_%d_%d" % (cch, lb)], [pyk], inc=True)
                    cp(k, "act" if lb % 2 else "dve", yst[:, cch, lb * 512:(lb + 1) * 512], py[:], [pyk], ["yst"])
            k.dma("sp", S["Yd"][1].rearrange("(c p) t -> p c t", p=128), yst[:], reads=["yst"], writes=["Yd1"])


def phase_gate(nc, k, C, I, S, l):
    with Ctx(nc, k, "g%d" % l) as cx:
        hT = cx.sb([128, 8, L], BF16, "hT")
        for tb in range(4):
            k.dma("sp", hT[:, :, tb * 512:(tb + 1) * 512], S["hTd"][:, :, tb * 512:(tb + 1) * 512], reads=["hTd"], writes=["hT%d" % tb])
        Y = cx.sb([128, 4, 2, L], BF16, "Y")
        for n in range(4):
            k.dma("act", Y[:, n], S["Yd"][n].rearrange("(c p) t -> p c t", p=128), reads=["Yd%d" % n], writes=["Y%d" % n])
        mT = cx.sb([128, 8, L], BF16, "mT")
        wout = cx.sb([128, 8, D], BF16, "wout")
        bg = cx.sb([128, 4, 8], F32, "bg")
        for n in range(4):
            k.dma("sp", bg[:, n, :], I["b_gate"][l, n * 1024:(n + 1) * 1024].rearrange("(dc p) -> p dc", p=128), writes=["bg"], allow_slow_non_contiguous=True)
        wgr = cx.rot_sb(2, [128, 4, 8, 128], BF16, "wg")
        wbr = cx.rot_sb(2, [128, 4, 2, 128], BF16, "wb")
        sgr = cx.rot_sb(2, [128, 512], F32, "sg")
        acr = cx.rot_sb(2, [128, 512], F32, "acc")
        tmr = cx.rot_sb(2, [128, 512], F32, "tmp")
        xr = cx.rot_sb(2, [128, D], F32, "x")
        pgr = cx.rot_ps(2, [128, 512], F32, "pg")
        ppr = cx.rot_ps(2, [128, 512], F32, "pp")
        por = cx.rot_ps(2, [128, 512], F32, "po")
        def load_gw(dc):
            wg, wgk = wgr.next(); wb, wbk = wbr.next()
            for n in range(4):
                col = O_GATE + n * 1024 + dc * 128
                k.dma("pool", wg[:, n], I["w_in"][l, :, col:col + 128].rearrange("(c p) m -> p c m", p=128), writes=[wgk])
                k.dma("pool", wb[:, n], I["w_br"][l, n, :, dc * 128:(dc + 1) * 128].rearrange("(c p) m -> p c m", p=128), writes=[wbk])
            return wg, wgk, wb, wbk

        nxt = load_gw(0)
        for j in range(2):
            k.dma("pool", wout[:, :, j * 512:(j + 1) * 512], I["w_out"][l, :, j * 512:(j + 1) * 512].rearrange("(c p) n -> p c n", p=128), writes=["wout"])
        for dc in range(8):
            wg, wgk, wb, wbk = nxt
            if dc + 1 < 8:
                nxt = load_gw(dc + 1)
            for tb in range(4):
                tsl = slice(tb * 512, (tb + 1) * 512)
                acc, ack = acr.next()
                for n in range(4):
                    pg, pgk = pgr.next(); pp, ppk = ppr.next()
                    for c in range(8):
                        mm(k, pg[:], wg[:, n, c, :], hT[:, c, tsl], c == 0, c == 7, [wgk, "hT%d" % tb], [pgk], inc=(c == 7))
                    for c in range(2):
                        mm(k, pp[:], wb[:, n, c, :], Y[:, n, c, tsl], c == 0, c == 1, [wbk, "Y%d" % n], [ppk], inc=(c == 1))
                    sg, sgk = sgr.next()
                    act(k, sg[:], pg[:], AF.Sigmoid, [pgk, "bg"], [sgk], bias=bg[:, n, dc:dc + 1])
                    if n == 0:
                        tt(k, "dve", acc[:], sg[:], pp[:], ALU.mult, [sgk, ppk], [ack])
                    else:
                        tm, tmk = tmr.next()
                        tt(k, "dve", tm[:], sg[:], pp[:], ALU.mult, [sgk, ppk], [tmk])
                        if n < 3:
                            tt(k, "pool", acc[:], acc[:], tm[:], ALU.add, [ack, tmk], [ack])
                        else:
                            tt(k, "pool", mT[:, dc, tsl], acc[:], tm[:], ALU.add, [ack, tmk], ["mT%d_%d" % (dc, tb)])
        for i in range(NT):
            xt, xk = xr.next()
            k.dma("sp", xt[:], S["Xd"][i * 128:(i + 1) * 128, :], reads=["Xd%d" % i], writes=[xk])
            for dh in range(2):
                po, pok = por.next()
                for dc in range(8):
                    mm(k, po[:], mT[:, dc, i * 128:(i + 1) * 128], wout[:, dc, dh * 512:(dh + 1) * 512], dc == 0, dc == 7,
                       ["mT%d_%d" % (dc, i // 4), "wout"], [pok], inc=(dc == 7))
                tt(k, "dve", xt[:, dh * 512:(dh + 1) * 512], xt[:, dh * 512:(dh + 1) * 512], po[:], ALU.add, [xk, pok], [xk])
            k.dma("sp", S["Xd"][i * 128:(i + 1) * 128, :], xt[:], reads=[xk], writes=["Xd%d" % i])


def phase_moe(nc, k, C, I, S, l):
    CAP = 256
    with Ctx(nc, k, "moe%d" % l) as cx:
        h2 = cx.sb([128, NT, D], BF16, "h2")
        aff = cx.sb([128, NT, 16], F32, "aff")
        posT = cx.sb([128, NT, 16], F32, "posT")
        with Ctx(nc, k, "moeR%d" % l) as c1:
            gt = bcast_load(k, c1, I["norm2_g"][l], D, "g")
            wr = c1.sb([128, 8, 16], F32, "wr")
            k.dma("sp", wr[:], I["w_router"][l].rearrange("(c p) e -> p c e", p=128), writes=["wr"])
            xr = c1.rot_sb(2, [128, D], F32, "x"); hfr = c1.rot_sb(2, [128, D], F32, "hf")
            hfTr = c1.rot_sb(2, [128, 8, 128], F32, "hfT")
            ssr = c1.rot_sb(2, [128, 1], F32, "ss")
            junk = c1.sb([128, D], F32, "junk")
            ptr = c1.rot_ps(2, [128, 4, 128], F32, "pt")
            plr = c1.rot_ps(2, [128, 16], F32, "pl")
            smr = c1.rot_sb(6, [128, 1], F32, "sm")
            exr = c1.rot_sb(2, [128, 16], F32, "ex")
            for i in range(NT):
                xt, xk = xr.next()
                k.dma("sp", xt[:], S["Xd"][i * 128:(i + 1) * 128, :], reads=["Xd%d" % i], writes=[xk])
                ss, sk = ssr.next()
                rms_rstd(k, xt[:], xk, junk[:], "junk", ss[:], sk, D)
                hf, hfk = hfr.next()
                stt(k, hf[:], xt[:], ss[:, 0:1], gt[:], ALU.mult, ALU.mult, [xk, sk, "g"], [hfk])
                cp(k, "act", h2[:, i, :], hf[:], [hfk], ["h2_%d" % i])
                hfT, hfTk = hfTr.next()
                for g in range(2):
                    pt, ptk = ptr.next()
                    for j in range(4):
                        c = g * 4 + j
                        tp(k, pt[:, j, :], hf[:, c * 128:(c + 1) * 128], C["identf"][:], [hfk, "identf"], [ptk], inc=(j == 3))
                    cp(k, "dve" if g else "act", hfT[:, g * 4:(g + 1) * 4, :], pt[:], [ptk], [hfTk])
                pl, plk = plr.next()
                for c in range(8):
                    mm(k, pl[:], hfT[:, c, :], wr[:, c, :], c == 0, c == 7, [hfTk, "wr"], [plk], inc=(c == 7))
                mx, mxk = smr.next(); sm, smk = smr.next(); rs, rsk = smr.next()
                k.op("dve", lambda e: e.reduce_max(out=mx[:], in_=pl[:], axis=AX.X), [plk], [mxk])
                ts(k, "dve", mx[:], mx[:], -1.0, None, ALU.mult, None, [mxk], [mxk])
                ex, exk = exr.next()
                act(k, ex[:], pl[:], AF.Exp, [plk, mxk], [exk, smk], bias=mx[:, 0:1], accum_out=sm[:])
                k.op("dve", lambda e: e.reciprocal(out=rs[:], in_=sm[:]), [smk], [rsk])
                ts(k, "dve", aff[:, i, :], ex[:], rs[:, 0:1], None, ALU.mult, None, [exk, rsk], ["aff%d" % i])
            affT = c1.sb([16, L], F32, "affT"); work = c1.sb([16, L], F32, "work")
            for g in range(4):
                pt, ptk = ptr.next()
                for j in range(4):
                    i = g * 4 + j
                    tp(k, pt[0:16, j, :], aff[:, i, :], C["identf"][:], ["aff%d" % i, "identf"], [ptk], inc=(j == 3))
                cp(k, "act", affT[:, g * 512:(g + 1) * 512], pt[0:16, :, :], [ptk], ["affT"])
            lo = c1.sb([16, 1], F32, "lo"); hi = c1.sb([16, 1], F32, "hi"); mid = c1.sb([16, 1], F32, "mid")
            cnt = c1.sb([16, 1], F32, "cnt"); ge = c1.sb([16, 1], F32, "ge"); dd = c1.sb([16, 1], F32, "dd")
            k.op("dve", lambda e: e.memset(lo[:], 0.0), [], ["lo"])
            k.op("dve", lambda e: e.memset(hi[:], 1.0), [], ["hi"])
            for it in range(30):
                tt(k, "dve", mid[:], lo[:], hi[:], ALU.add, ["lo", "hi"], ["mid"])
                ts(k, "dve", mid[:], mid[:], 0.5, None, ALU.mult, None, ["mid"], ["mid"])
                k.op("dve", lambda e: e.tensor_scalar(out=work[:], in0=affT[:], scalar1=mid[:, 0:1], scalar2=0.0, op0=ALU.is_ge, op1=ALU.add, accum_out=cnt[:]),
                     ["affT", "mid"], ["work", "cnt"])
                ts(k, "dve", ge[:], cnt[:], float(CAP), None, ALU.is_ge, None, ["cnt"], ["ge"])
                tt(k, "dve", dd[:], mid[:], lo[:], ALU.subtract, ["mid", "lo"], ["dd"])
                stt(k, lo[:], dd[:], ge[:, 0:1], lo[:], ALU.mult, ALU.add, ["dd", "ge", "lo"], ["lo"])
                tt(k, "dve", dd[:], hi[:], mid[:], ALU.subtract, ["hi", "mid"], ["dd"])
                stt(k, hi[:], dd[:], ge[:, 0:1], mid[:], ALU.mult, ALU.add, ["dd", "ge", "mid"], ["hi"])
            mask = c1.sb([16, L], F32, "mask"); ones = c1.sb([16, L], F32, "ones"); pos = c1.sb([16, L], F32, "pos")
            k.op("pool", lambda e: e.memset(ones[:], 1.0), [], ["ones"])
            ts(k, "dve", mask[:], affT[:], lo[:, 0:1], None, ALU.is_ge, None, ["affT", "lo"], ["mask"])
            k.op("dve", lambda e: e.tensor_tensor_scan(out=pos[:], data0=ones[:], data1=mask[:], initial=0.0, op0=ALU.mult, op1=ALU.add), ["ones", "mask"], ["pos"])
            tt(k, "dve", pos[:], pos[:], mask[:], ALU.mult, ["pos", "mask"], ["pos"])
            ts(k, "dve", pos[:], pos[:], -1.0, None, ALU.add, None, ["pos"], ["pos"])
            ppos = c1.rot_ps(1, [128, NT, 16], F32, "ppos")
            pp_, ppk = ppos.next()
            for i in range(NT):
                tp(k, pp_[:, i, :], pos[:, i * 128:(i + 1) * 128], C["identf"][0:16, 0:16], ["pos", "identf"], [ppk], inc=(i == NT - 1))
            cp(k, "act", posT[:], pp_[:], [ppk], ["posT"])
            sel = c1.sb([16, 16, 128], BF16, "sel")
            k.dma("pool", sel[:], I["sel16"], writes=["sel"])
            posb = c1.sb([16, L], BF16, "posb")
            cp(k, "act", posb[:], pos[:], ["pos"], ["posb"])
            iotap = c1.sb([128, 2], F32, "iotap")
            k.dma("sp", iotap[:], I["iotap"], writes=["iotap"])
            pbr = c1.rot_ps(2, [128, 512], F32, "pb")
            str_ = c1.rot_sb(4, [128, L], BF16, "st")
            for e in range(16):
                st0, st0k = str_.next(); st1, st1k = str_.next()
                for tb in range(4):
                    pb, pbk = pbr.next()
                    mm(k, pb[:], sel[:, e, :], posb[:, tb * 512:(tb + 1) * 512], True, True, ["sel", "posb"], [pbk])
                    ts(k, "dve", st0[:, tb * 512:(tb + 1) * 512], pb[:], iotap[:, 0:1], None, ALU.is_equal, None, [pbk, "iotap"], [st0k])
                    ts(k, "dve", st1[:, tb * 512:(tb + 1) * 512], pb[:], iotap[:, 1:2], None, ALU.is_equal, None, [pbk, "iotap"], [st1k])
                k.dma("sp", S["ST_d"][e, 0:128, :], st0[:], reads=[st0k], writes=["ST_d%d" % e])
                k.dma("sp", S["ST_d"][e, 128:256, :], st1[:], reads=[st1k], writes=["ST_d%d" % e])
        with Ctx(nc, k, "moeF%d" % l) as c2:
            iota = c2.sb([128, 256], F32, "iota")
            k.dma("sp", iota[:], I["iota256"], writes=["iota"])
            affhl = c2.sb([128, NT, 16, 2], BF16, "affhl")
            afft = c2.sb([128, NT, 16], F32, "afft")
            allaff = ["aff%d" % i for i in range(NT)]
            cp(k, "dve", affhl[:, :, :, 0], aff[:], allaff, ["affhl"])
            cp(k, "dve", afft[:], affhl[:, :, :, 0], ["affhl"], ["afft"])
            tt(k, "dve", afft[:], aff[:], afft[:], ALU.subtract, allaff + ["afft"], ["afft"])
            cp(k, "dve", affhl[:, :, :, 1], afft[:], ["afft"], ["affhl"])
            wgr = c2.rot_sb(2, [128, 8, D], BF16, "wg"); wur = c2.rot_sb(2, [128, 8, D], BF16, "wu"); wdr = c2.rot_sb(2, [128, 8, D], BF16, "wd")
            ser = c2.rot_sb(2, [128, NT, 256], BF16, "se")
            xer = c2.rot_sb(2, [128, 8, 256], BF16, "xe")
            acr = c2.rot_sb(2, [128, 8, 256], BF16, "ac")
            yer = c2.rot_sb(2, [128, 2, D], BF16, "ye")
            asr = c2.rot_sb(2, [128, 2], F32, "as")
            as2r = c2.rot_sb(2, [128, 2, 2], F32, "as2")
            sgr = c2.rot_sb(2, [128, 256], F32, "sg")
            pxr = c2.rot_ps(2, [128, 256], F32, "px")
            par = c2.rot_ps(1, [128, 2, 2], F32, "pa")
            pgr = c2.rot_ps(2, [128, 2, 256], F32, "pgu")
            pyr = c2.rot_ps(2, [128, 512], F32, "py")
            allh2 = ["h2_%d" % i for i in range(NT)]
            def load_ew(e):
                wg, wgk = wgr.next(); wu, wuk = wur.next(); wd, wdk = wdr.next()
                for (wt_, wk_, nm) in ((wg, wgk, "w_e_gate"), (wu, wuk, "w_e_up"), (wd, wdk, "w_e_down")):
                    k.dma("pool", wt_[:], I[nm][l, e].rearrange("(c p) f -> p c f", p=128), writes=[wk_])
                return wg, wgk, wu, wuk, wd, wdk

            nxt = load_ew(0)
            for e in range(16):
                wg, wgk, wu, wuk, wd, wdk = nxt
                if e + 1 < 16:
                    nxt = load_ew(e + 1)
                se, sek = ser.next()
                for c in range(NT):
                    ts(k, "dve", se[:, c, :], iota[:], posT[:, c, e:e + 1], None, ALU.is_equal, None, ["iota", "posT"], [sek])
                xe, xek = xer.next()
                for dk in range(8):
                    px, pxk = pxr.next()
                    for c in range(NT):
                        mm(k, px[:], h2[:, c, dk * 128:(dk + 1) * 128], se[:, c, :], c == 0, c == NT - 1, [allh2[c], sek], [pxk], inc=(c == NT - 1))
                    cp(k, "act" if dk % 2 else "dve", xe[:, dk, :], px[:], [pxk], [xek])
                pa, pak = par.next()
                for jc in range(2):
                    for c in range(NT):
                        mm(k, pa[:, jc, :], se[:, c, jc * 128:(jc + 1) * 128], affhl[:, c, e, :], c == 0, c == NT - 1, [sek, "affhl"], [pak], inc=(c == NT - 1))
                as_, ask = asr.next()
                as2, as2k = as2r.next()
                cp(k, "act", as2[:], pa[:], [pak], [as2k])
                tt(k, "dve", as_[:], as2[:, :, 0], as2[:, :, 1], ALU.add, [as2k], [ask])
                ac, ack = acr.next()
                for f in range(8):
                    pgu, pgk = pgr.next()
                    for dk in range(8):
                        mm(k, pgu[:, 0, :], wg[:, dk, f * 128:(f + 1) * 128], xe[:, dk, :], dk == 0, dk == 7, [wgk, xek], [pgk], inc=False)
                    for dk in range(8):
                        mm(k, pgu[:, 1, :], wu[:, dk, f * 128:(f + 1) * 128], xe[:, dk, :], dk == 0, dk == 7, [wuk, xek], [pgk], inc=(dk == 7))
                    sg, sgk = sgr.next()
                    act(k, sg[:], pgu[:, 0, :], AF.Silu, [pgk], [sgk])
                    tt(k, "dve", ac[:, f, :], sg[:], pgu[:, 1, :], ALU.mult, [sgk, pgk], [ack])
                ye, yek = yer.next()
                for jc in range(2):
                    for dh in range(2):
                        py, pyk = pyr.next()
                        for f in range(8):
                            mm(k, py[:], ac[:, f, jc * 128:(jc + 1) * 128], wd[:, f, dh * 512:(dh + 1) * 512], f == 0, f == 7, [ack, wdk], [pyk], inc=(f == 7))
                        ts(k, "dve", ye[:, jc, dh * 512:(dh + 1) * 512], py[:], as_[:, jc:jc + 1], None, ALU.mult, None, [pyk, ask], [yek])
                k.dma("sp", S["ye_d"][e].rearrange("(jc p) d -> p jc d", p=128), ye[:], reads=[yek], writes=["ye_d%d" % e])
        with Ctx(nc, k, "moeS%d" % l) as c3:
            yeall = c3.sb([128, 32, D], BF16, "yeall")
            for e in range(16):
                k.dma("act" if e % 2 else "sp", yeall[:, 2 * e:2 * e + 2, :], S["ye_d"][e].rearrange("(jc p) d -> p jc d", p=128), reads=["ye_d%d" % e], writes=["yeall%d" % e])
            STr = c3.rot_sb(2, [128, 32, 512], BF16, "ST")
            xr = c3.rot_sb(2, [128, D], F32, "x")
            por = c3.rot_ps(2, [128, 512], F32, "po")
            def load_ST(tb):
                ST, STk = STr.next()
                for e in range(16):
                    k.dma("act" if e % 2 else "sp", ST[:, 2 * e:2 * e + 2, :], S["ST_d"][e, :, tb * 512:(tb + 1) * 512].rearrange("(jc p) t -> p jc t", p=128),
                          reads=["ST_d%d" % e], writes=["%s_%d" % (STk, e)])
                return ST, STk

            nxt = load_ST(0)
            for tb in range(4):
                ST, STk = nxt
                if tb + 1 < 4:
                    nxt = load_ST(tb + 1)
                for j in range(4):
                    i = tb * 4 + j
                    xt, xk = xr.next()
                    k.dma("sp", xt[:], S["Xd"][i * 128:(i + 1) * 128, :], reads=["Xd%d" % i], writes=[xk])
                    for dh in range(2):
                        po, pok = por.next()
                        for m in range(32):
                            mm(k, po[:], ST[:, m, j * 128:(j + 1) * 128], yeall[:, m, dh * 512:(dh + 1) * 512], m == 0, m == 31, ["%s_%d" % (STk, m // 2), "yeall%d" % (m // 2)], [pok], inc=(m == 31))
                        tt(k, "dve", xt[:, dh * 512:(dh + 1) * 512], xt[:, dh * 512:(dh + 1) * 512], po[:], ALU.add, [xk, pok], [xk])
                    k.dma("sp", S["Xd"][i * 128:(i + 1) * 128, :], xt[:], reads=[xk], writes=["Xd%d" % i])


def phase_ple(nc, k, C, I, S, l, out=None):
    with Ctx(nc, k, "ple%d" % l) as cx:
        h3T = cx.sb([128, 8, L], BF16, "h3T")
        phase_norm_T(nc, k, C, "n3_%d" % l, S["Xd"], lambda i: "Xd%d" % i, I["norm3_g"][l], h3T, lambda i: "h3T%d" % i)
        wpg = cx.sb([128, 8, D], BF16, "wpg"); wpp = cx.sb([128, 2, D], BF16, "wpp")
        for j in range(2):
            k.dma("pool", wpg[:, :, j * 512:(j + 1) * 512], I["w_ple_gate"][l, :, j * 512:(j + 1) * 512].rearrange("(c p) n -> p c n", p=128), writes=["wpg"])
        k.dma("pool", wpp[:], I["w_ple_proj"][l].rearrange("(c p) n -> p c n", p=128), writes=["wpp"])
        pinr = cx.rot_sb(2, [128, 256], F32, "pin"); pbr = cx.rot_sb(2, [128, 256], BF16, "pb")
        pTr = cx.rot_sb(2, [128, 2, 128], BF16, "pT")
        xr = cx.rot_sb(2, [128, D], F32, "x")
        sgr = cx.rot_sb(2, [128, 512], F32, "sg"); tmr = cx.rot_sb(2, [128, 512], F32, "tm")
        ptr = cx.rot_ps(2, [128, 2, 128], BF16, "pt")
        pgr = cx.rot_ps(2, [128, 512], F32, "pg"); ppr = cx.rot_ps(2, [128, 512], F32, "pp")
        if out is not None:
            gtf = bcast_load(k, cx, I["final_g"], D, "gf")
            yr = cx.rot_sb(2, [128, D], F32, "y")
            ssr = cx.rot_sb(2, [128, 1], F32, "ssf")
            junk = cx.sb([128, D], F32, "junkf")
        for i in range(NT):
            pin, pink = pinr.next()
            k.dma("act", pin[:], I["p"][l, i * 128:(i + 1) * 128, :], writes=[pink])
            pb, pbk = pbr.next()
            cp(k, "pool", pb[:], pin[:], [pink], [pbk])
            pt, ptk = ptr.next()
            for c in range(2):
                tp(k, pt[:, c, :], pb[:, c * 128:(c + 1) * 128], C["identb"][:], [pbk, "identb"], [ptk], inc=(c == 1))
            pT, pTk = pTr.next()
            cp(k, "act", pT[:], pt[:], [ptk], [pTk])
            xt, xk = xr.next()
            k.dma("sp", xt[:], S["Xd"][i * 128:(i + 1) * 128, :], reads=["Xd%d" % i], writes=[xk])
            for dh in range(2):
                dsl = slice(dh * 512, (dh + 1) * 512)
                pg, pgk = pgr.next(); pp, ppk = ppr.next()
                for c in range(8):
                    mm(k, pg[:], h3T[:, c, i * 128:(i + 1) * 128], wpg[:, c, dsl], c == 0, c == 7, ["wpg"], [pgk], inc=(c == 7))
                for c in range(2):
                    mm(k, pp[:], pT[:, c, :], wpp[:, c, dsl], c == 0, c == 1, [pTk, "wpp"], [ppk], inc=(c == 1))
                sg, sgk = sgr.next()
                act(k, sg[:], pg[:], AF.Sigmoid, [pgk], [sgk])
                tm, tmk = tmr.next()
                tt(k, "dve", tm[:], sg[:], pp[:], ALU.mult, [sgk, ppk], [tmk])
                tt(k, "pool", xt[:, dsl], xt[:, dsl], tm[:], ALU.add, [xk, tmk], [xk])
            if out is None:
                k.dma("sp", S["Xd"][i * 128:(i + 1) * 128, :], xt[:], reads=[xk], writes=["Xd%d" % i])
            else:
                ss, sk = ssr.next()
                rms_rstd(k, xt[:], xk, junk[:], "junkf", ss[:], sk, D)
                y, yk = yr.next()
                stt(k, y[:], xt[:], ss[:, 0:1], gtf[:], ALU.mult, ALU.mult, [xk, sk, "gf"], [yk])
                k.dma("act", out[i * 128:(i + 1) * 128, :], y[:], reads=[yk], writes=["out%d" % i])


def phase_final(nc, k, C, I, S, out):
    with Ctx(nc, k, "fin") as cx:
        gt = bcast_load(k, cx, I["final_g"], D, "g")
        xr = cx.rot_sb(2, [128, D], F32, "x"); yr = cx.rot_sb(2, [128, D], F32, "y")
        ssr = cx.rot_sb(2, [128, 1], F32, "ss")
        junk = cx.sb([128, D], F32, "junk")
        for i in range(NT):
            xt, xk = xr.next()
            k.dma("sp", xt[:], S["Xd"][i * 128:(i + 1) * 128, :], reads=["Xd%d" % i], writes=[xk])
            ss, sk = ssr.next()
            rms_rstd(k, xt[:], xk, junk[:], "junk", ss[:], sk, D)
            y, yk = yr.next()
            stt(k, y[:], xt[:], ss[:, 0:1], gt[:], ALU.mult, ALU.mult, [xk, sk, "g"], [yk])
            k.dma("act", out[i * 128:(i + 1) * 128, :], y[:], reads=[yk], writes=["out%d" % i])


def build_program(dbg=False, nlayers=DEPTH, phases=None, track=None):
    bf = ml_dtypes.bfloat16
    nc = bass.Bass("TRN2", target_bir_lowering=False)
    k = KB(nc)
    k.track = track
    I = {}

    def din(name, shape, dt=F32):
        I[name] = nc.dram_tensor(name, list(shape), dt, kind="ExternalInput").ap()

    din("x", (L, D)); din("p", (2, L, 256))
    for n, s in WEIGHT_SHAPES.items():
        din(n, s)
    for n, v in const_specs().items():
        din(n, v.shape, BF16 if v.dtype == bf else F32)
    out = nc.dram_tensor("out", [L, D], F32, kind="ExternalOutput").ap()
    S = {}

    def dscr(name, shape, dt):
        S[name] = nc.dram_tensor(name, list(shape), dt, kind=("ExternalOutput" if dbg else "Internal")).ap()

    dscr("Xd", (L, D), F32)
    dscr("hTd", (128, 8, L), BF16)
    dscr("Yd", (4, 256, L), BF16)
    dscr("x1Td", (256, L), F32); dscr("x2Td", (256, L), F32); dscr("vTd", (256, L), F32)
    dscr("v_d", (L, 256), BF16); dscr("z2Td", (256, L), F32); dscr("z2_d", (L, 256), BF16)
    dscr("fn_d", (2, L, 256), BF16)
    dscr("KAd", (2, L, 256), F32); dscr("KBd", (2, L, 256), F32); dscr("KNd", (2, 256), F32)
    dscr("bias_d", (60, 4096), F32)
    dscr("ye_d", (16, 256, D), BF16); dscr("ST_d", (16, 256, L), BF16)

    def on(ph):
        return phases is None or ph in phases

    with Ctx(nc, k, "glob") as g:
        C = {}
        C["identf"] = g.sb([128, 128], F32, "identf")
        C["identb"] = g.sb([128, 128], BF16, "identb")
        k.dma("sp", C["identf"][:], I["ident"], writes=["identf"])
        cp(k, "dve", C["identb"][:], C["identf"][:], ["identf"], ["identb"])
        k.dma("sp", S["Xd"], I["x"], writes=["Xd%d" % i for i in range(NT)])
        k.barrier()
        for l in range(nlayers):
            with Ctx(nc, k, "mix%d" % l) as mx:
                hT = mx.sb([128, 8, L], BF16, "hT")
                phase_norm_T(nc, k, C, "n1_%d" % l, S["Xd"], lambda i: "Xd%d" % i, I["norm1_g"][l], hT, lambda i: "hT%d" % i)
                if on("mla"):
                    phase_mla(nc, k, C, I, S, l, hT)
                if on("na"):
                    phase_na(nc, k, C, I, S, l, hT)
                if on("hy"):
                    phase_hyfn_prep(nc, k, C, I, S, l, hT)
                k.dma("sp", S["hTd"], hT[:], reads=["hT%d" % i for i in range(NT)], writes=["hTd"])
            if on("hy"):
                phase_dft(nc, k, C, I, S, l)
            if on("gate"):
                phase_gate(nc, k, C, I, S, l)
            if on("moe"):
                phase_moe(nc, k, C, I, S, l)
            fuse_final = on("final") and on("ple") and l == nlayers - 1
            if on("ple"):
                phase_ple(nc, k, C, I, S, l, out if fuse_final else None)
        if on("final") and not on("ple"):
            phase_final(nc, k, C, I, S, out)
        k.finish("sp")
    return nc


def make_in_maps(inputs, cores):
    consts = const_specs()
    maps = []
    for b in cores:
        m = {"x": np.ascontiguousarray(inputs["x"][b]), "p": np.ascontiguousarray(inputs["p"][:, b])}
        for n in WEIGHT_SHAPES:
            m[n] = np.ascontiguousarray(inputs[n])
        m.update(consts)
        maps.append(m)
    return maps


def kernel(**inputs):
    inputs = {k_: np.asarray(v) for k_, v in inputs.items()}
    nc = build_program()
    maps = make_in_maps(inputs, list(range(8)))
    res = run_bass_kernel_spmd(nc, maps, core_ids=list(range(8)))
    return np.stack([np.asarray(r["out"]) for r in res.results], axis=0).astype(np.float32)
```

```python
import math
from contextlib import ExitStack
import numpy as np
import ml_dtypes
import concourse.bass as bass
import concourse.mybir as mybir
from concourse.bass_utils import run_bass_kernel_spmd

F32 = mybir.dt.float32
BF16 = mybir.dt.bfloat16
ALU = mybir.AluOpType
AF = mybir.ActivationFunctionType
AX = mybir.AxisListType

L = 2048
D = 1024
NT = 16
DEPTH = 2
EPS = 1e-6
IN_COLS = 6304
O_HY, O_FN, O_CQ, O_CKV, O_KPE, O_NA, O_GATE = 0, 768, 1024, 1280, 1408, 1440, 2208
NFFT = 4096


class KB:
    N_DMA_SEMS = 28
    N_HW = 20

    def __init__(self, nc, same_engine_sync=True):
        self.nc = nc
        self.eng = {"pe": nc.tensor, "act": nc.scalar, "dve": nc.vector,
                    "pool": nc.gpsimd, "sp": nc.sync}
        self.sem = {k: nc.alloc_semaphore("s_" + k) for k in ("pe", "act", "dve", "pool")}
        self.cnt = {k: 0 for k in self.sem}
        self.pending = {k: [] for k in self.sem}
        self.seen = {k: {} for k in self.eng}
        self.dsem = [nc.alloc_semaphore("d%d" % i) for i in range(self.N_DMA_SEMS)]
        self.dval = [0] * self.N_DMA_SEMS
        self.drr = 0
        self.drr_sw = 0
        self.res = {}
        self.ses = same_engine_sync
        self.n_ins = 0
        self.n_wait = 0
        self.track = None
        self.phase = ""
        self.excl = set()

    def _r(self, key):
        r = self.res.get(key)
        if r is None:
            r = {"w": None, "r": []}
            self.res[key] = r
        return r

    def _collect(self, reads, writes, ename=None):
        st = []
        for k in reads:
            r = self._r(k)
            if r["w"] is not None:
                st.append(r["w"])
        for k in writes:
            r = self._r(k)
            if r["w"] is not None:
                st.append(r["w"])
            st.extend(r["r"])
        return st

    def _emit_waits(self, ename, stamps, skip_same=False):
        need = {}
        for (kind, idx, val) in stamps:
            if kind == "e":
                if idx == ename and (skip_same or not self.ses):
                    continue
                assert val <= self.cnt[idx], "dependency on pending stamp %s %d" % (idx, val)
            key = (kind, idx)
            if self.seen[ename].get(key, 0) >= val:
                continue
            if need.get(key, 0) < val:
                need[key] = val
        e = self.eng[ename]
        for (kind, idx), val in need.items():
            s = self.sem[idx] if kind == "e" else self.dsem[idx]
            e.wait_ge(s, val)
            self.seen[ename][(kind, idx)] = val
            self.n_wait += 1

    def _stamp(self, reads, writes, stamp):
        for k in reads:
            rr = self._r(k)["r"]
            rr.append(stamp)
            if len(rr) > 64:
                best = {}
                for s in rr:
                    kk = (s[0], s[1])
                    if best.get(kk, (0, 0, 0))[2] < s[2]:
                        best[kk] = s
                rr[:] = list(best.values())
        for k in writes:
            r = self._r(k)
            r["w"] = stamp
            r["r"] = []

    def op(self, ename, fn, reads=(), writes=(), inc=True, skip_same=False):
        reads = list(reads); writes = list(writes)
        writes = writes + [r for r in reads if r in self.excl and r not in writes]
        self._emit_waits(ename, self._collect(reads, writes, ename), skip_same=skip_same)
        ins = fn(self.eng[ename])
        self.n_ins += 1
        if self.track is not None:
            try:
                self.track[str(ins.ins.name)] = self.phase
            except Exception:
                pass
        if inc:
            ins.then_inc(self.sem[ename], 1)
            self.cnt[ename] += 1
            stamp = ("e", ename, self.cnt[ename])
            for (rd, wr) in self.pending[ename]:
                self._stamp(rd, wr, stamp)
            self.pending[ename] = []
            self._stamp(reads, writes, stamp)
        else:
            self.pending[ename].append((reads, writes))
        return ins

    def dma(self, qname, out, in_, reads=(), writes=(), **kw):
        reads = list(reads); writes = list(writes)
        if qname == "pool":
            i = self.N_HW + self.drr_sw
            self.drr_sw = (self.drr_sw + 1) % (self.N_DMA_SEMS - self.N_HW)
        else:
            i = self.drr
            self.drr = (self.drr + 1) % self.N_HW
        st = self._collect(reads, writes)
        if self.dval[i] > 0:
            st.append(("d", i, self.dval[i]))
        self._emit_waits(qname, st, skip_same=True)
        ins = self.eng[qname].dma_start(out=out, in_=in_, **kw)
        self.n_ins += 1
        self.dval[i] += 16
        ins.then_inc(self.dsem[i], 16)
        self._stamp(reads, writes, ("d", i, self.dval[i]))
        return ins

    def barrier(self):
        st = [("e", k, v) for k, v in self.cnt.items() if v > 0]
        st += [("d", i, v) for i, v in enumerate(self.dval) if v > 0]
        for k in self.pending:
            assert not self.pending[k]
        for ename in self.eng:
            self._emit_waits(ename, [s for s in st if not (s[0] == "e" and s[1] == ename)])
        self.res = {}

    def finish(self, qname="sp"):
        st = [("e", k, v) for k, v in self.cnt.items() if v > 0]
        st += [("d", i, v) for i, v in enumerate(self.dval) if v > 0]
        self._emit_waits(qname, st)


class Rot:
    def __init__(self, tiles, name):
        self.tiles = tiles
        self.name = name
        self.i = 0

    def next(self):
        j = self.i % len(self.tiles)
        self.i += 1
        return self.tiles[j], "%s#%d" % (self.name, j)


class Ctx:
    def __init__(self, nc, k, tag):
        self.nc, self.k, self.tag = nc, k, tag
        self.es = ExitStack()
        self.n = 0

    def __enter__(self):
        self.es.__enter__()
        self.prev_phase = self.k.phase
        self.k.phase = self.tag
        return self

    def __exit__(self, *a):
        if a[0] is None:
            self.k.barrier()
        self.k.phase = self.prev_phase
        return self.es.__exit__(*a)

    def sb(self, shape, dt, name=None):
        self.n += 1
        return self.es.enter_context(self.nc.sbuf_tensor("%s_%s%d" % (self.tag, name or "t", self.n), list(shape), dt))

    def ps(self, shape, dt, name=None):
        self.n += 1
        esz = 2 if dt == BF16 else 4
        n = 1
        for d in shape[1:]:
            n *= d
        assert n * esz <= 2048, shape
        full = self.es.enter_context(self.nc.psum_tensor("%s_%s%d" % (self.tag, name or "p", self.n), [128, 2048 // esz], dt))
        v = full[0:shape[0], 0:n]
        if len(shape) == 3:
            v = v.rearrange("p (a b) -> p a b", a=shape[1])
        return v

    def rot_sb(self, n, shape, dt, name):
        return Rot([self.sb(shape, dt, name) for _ in range(n)], self.tag + name)

    def rot_ps(self, n, shape, dt, name):
        r = Rot([self.ps(shape, dt, name) for _ in range(n)], self.tag + name)
        for j in range(n):
            self.k.excl.add("%s#%d" % (r.name, j))
        return r


def host_consts():
    f32 = np.float32
    bf = ml_dtypes.bfloat16
    c = {}
    t = np.arange(L, dtype=np.int64)
    m = (np.outer(t, t) % NFFT).astype(np.float64) * (2.0 * np.pi / NFFT)
    c["dftA"] = np.cos(m).astype(bf)
    c["dftB"] = np.sin(m).astype(bf)
    c["ident"] = np.eye(128, dtype=f32)
    sg = np.where(np.arange(128) % 2 == 0, 1.0, -1.0)
    c["signp"] = sg.astype(f32).reshape(128, 1)
    c["altcol"] = sg.astype(f32).reshape(128, 1)
    c["altrow"] = np.where(t % 2 == 0, 1.0, -1.0).astype(f32).reshape(1, L)
    t01 = np.linspace(0.0, 1.0, L, dtype=f32)[:, None]
    bands = np.linspace(1e-4, 15, 16, dtype=f32)
    ang = f32(2.0 * math.pi) * np.arange(L, dtype=f32)[:, None] * bands / f32(L)
    z = np.concatenate([t01, np.cos(ang), -np.sin(ang)], axis=-1).astype(f32)
    c["hy_zT"] = np.ascontiguousarray(z.T)
    max_decay = math.log(1e-2) / 0.3
    min_decay = math.log(1e-2) / 1.5
    deltas = np.linspace(min_decay, max_decay, 256, dtype=f32)
    c["hy_win"] = np.exp(-t01 * np.abs(deltas)).astype(f32)
    inv = (10000.0 ** (-np.arange(0, 32, 2, dtype=f32) / f32(32))).astype(f32)
    ra = np.arange(L, dtype=f32)[:, None] * inv
    rc_, rs_ = np.cos(ra).astype(f32), np.sin(ra).astype(f32)
    cs2 = np.concatenate([rc_, rc_], axis=1)
    sn2 = np.concatenate([-rs_, rs_], axis=1)
    c["rope_cs2"] = np.ascontiguousarray(cs2.reshape(NT, 128, 32).transpose(1, 0, 2))
    c["rope_sn2"] = np.ascontiguousarray(sn2.reshape(NT, 128, 32).transpose(1, 0, 2))
    qs = f32(96.0 ** -0.5)
    c["rope_cs2q"] = np.ascontiguousarray(np.repeat((cs2 * qs).reshape(NT, 128, 1, 32), 4, axis=2).transpose(1, 0, 2, 3))
    c["rope_sn2q"] = np.ascontiguousarray(np.repeat((sn2 * qs).reshape(NT, 128, 1, 32), 4, axis=2).transpose(1, 0, 2, 3))
    cc = np.arange(64)
    a64 = 2.0 * np.pi * (np.outer(cc, cc) % 64) / 64.0
    nrm = 1.0 / math.sqrt(L * 64.0)
    C = np.zeros((256, 256)); S = np.zeros((256, 256))
    for g in range(4):
        C[g * 64:(g + 1) * 64, g * 64:(g + 1) * 64] = np.cos(a64) * nrm
        S[g * 64:(g + 1) * 64, g * 64:(g + 1) * 64] = -np.sin(a64) * nrm
    c["fn_C"] = C.astype(f32)
    c["fn_S"] = S.astype(f32)
    E = np.zeros((32, 64, 64), dtype=f32)
    kc = np.arange(64)[:, None]; qc = np.arange(64)[None, :]
    dc = np.clip(kc - qc + 15, 0, 30)
    for b in range(31):
        E[b] = (dc == b)
    cs = np.clip(qc - 8, 0, 48)
    ok = (kc >= cs) & (kc < cs + 16)
    E[31] = np.where(ok, 0.0, -30000.0)
    c["na_E"] = E.reshape(32, 4096)
    c["iota256"] = np.tile(np.arange(256, dtype=f32)[None, :], (128, 1))
    c["iotap"] = np.stack([np.arange(128), np.arange(128) + 128], axis=1).astype(f32)
    sel = np.zeros((16, 16, 128), dtype=f32)
    for e in range(16):
        sel[e, e, :] = 1.0
    c["sel16"] = sel
    c["ones_f"] = np.ones((128, 128), dtype=f32)
    return c


CONST_SPECS = None


def const_specs():
    global CONST_SPECS
    if CONST_SPECS is None:
        CONST_SPECS = host_consts()
    return CONST_SPECS


WEIGHT_SHAPES = {
    "norm1_g": (2, 1024), "w_in": (2, 1024, 6304), "b_gate": (2, 4096), "hy_conv_w": (2, 3, 768),
    "hy_conv_b": (2, 768), "hf_w1": (2, 33, 64), "hf_b1": (2, 64), "hf_freq": (2, 2, 64),
    "hf_w2": (2, 64, 64), "hf_b2": (2, 64), "hf_w3": (2, 64, 1024), "hy_skip": (2, 2, 256),
    "q_norm_g": (2, 256), "w_uq": (2, 256, 384), "kv_norm_g": (2, 128), "w_ukv": (2, 128, 512),
    "rpb": (2, 4, 15, 31), "w_br": (2, 4, 256, 1024), "w_out": (2, 1024, 1024), "norm2_g": (2, 1024),
    "w_router": (2, 1024, 16), "w_e_gate": (2, 16, 1024, 1024), "w_e_up": (2, 16, 1024, 1024),
    "w_e_down": (2, 16, 1024, 1024), "norm3_g": (2, 1024), "w_ple_gate": (2, 1024, 1024),
    "w_ple_proj": (2, 256, 1024), "final_g": (1024,),
}


def mm(k, out, lhsT, rhs, start, stop, reads, writes, inc=True):
    return k.op("pe", lambda e: e.matmul(out, lhsT=lhsT, rhs=rhs, start=start, stop=stop), reads, writes, inc=inc)


def tp(k, out, in_, ident, reads, writes, inc=True):
    return k.op("pe", lambda e: e.transpose(out=out, in_=in_, identity=ident), reads, writes, inc=inc)


def cp(k, eng, out, in_, reads, writes):
    if eng == "act":
        return k.op("act", lambda e: e.copy(out=out, in_=in_), reads, writes)
    return k.op(eng, lambda e: e.tensor_copy(out=out, in_=in_), reads, writes)


def tt(k, eng, out, in0, in1, op, reads, writes):
    return k.op(eng, lambda e: e.tensor_tensor(out=out, in0=in0, in1=in1, op=op), reads, writes)


def ts(k, eng, out, in0, s1, s2, op0, op1, reads, writes):
    if op1 is None:
        return k.op(eng, lambda e: e.tensor_scalar(out=out, in0=in0, scalar1=s1, scalar2=None, op0=op0), reads, writes)
    return k.op(eng, lambda e: e.tensor_scalar(out=out, in0=in0, scalar1=s1, scalar2=s2, op0=op0, op1=op1), reads, writes)


def stt(k, out, in0, scalar, in1, op0, op1, reads, writes):
    return k.op("dve", lambda e: e.scalar_tensor_tensor(out=out, in0=in0, scalar=scalar, in1=in1, op0=op0, op1=op1), reads, writes)


def act(k, out, in_, func, reads, writes, **kw):
    return k.op("act", lambda e: e.activation(out=out, in_=in_, func=func, **kw), reads, writes)


def rms_rstd(k, src, skey, junk, jkey, ss, sskey, n):
    act(k, junk, src, AF.Square, [skey], [jkey, sskey], accum_out=ss)
    ts(k, "dve", ss, ss, 1.0 / n, EPS, ALU.mult, ALU.add, [sskey], [sskey])
    act(k, ss, ss, AF.Sqrt, [sskey], [sskey])
    k.op("dve", lambda e: e.reciprocal(out=ss, in_=ss), [sskey], [sskey])


def bcast_load(k, cx, vec_ap, n, key, q="sp"):
    t = cx.sb([128, n], F32, "bc")
    k.dma(q, t[:], vec_ap.partition_broadcast(128), writes=[key])
    return t


def t_to_f(k, cx, C, src, skeys, dst_dram, dkey, nch=2):
    stage = cx.sb([128, nch, L], BF16, "tfst")
    pr = cx.rot_ps(2, [128, nch, 128], BF16, "tfp")
    for i in range(NT):
        pt, pk = pr.next()
        for c in range(nch):
            tp(k, pt[:, c, :], src[:, i, c * 128:(c + 1) * 128], C["identb"][:], [skeys(i), "identb"], [pk], inc=(c == nch - 1))
        cp(k, "act" if i % 2 else "dve", stage[:, :, i * 128:(i + 1) * 128], pt[:], [pk], ["tfst%d" % i])
    k.dma("sp", dst_dram.rearrange("(c p) t -> p c t", p=128), stage[:], reads=["tfst%d" % i for i in range(NT)], writes=[dkey])


def phase_norm_T(nc, k, C, tag, Xsrc, xkey, g_ap, hT, hkey):
    with Ctx(nc, k, tag) as cx:
        gt = bcast_load(k, cx, g_ap, D, "g")
        xr = cx.rot_sb(2, [128, D], F32, "x")
        hr = cx.rot_sb(2, [128, D], BF16, "h")
        ssr = cx.rot_sb(2, [128, 1], F32, "ss")
        junk = cx.sb([128, D], F32, "junk")
        ptr = cx.rot_ps(2, [128, 8, 128], BF16, "pt")
        for i in range(NT):
            xt, xk = xr.next()
            k.dma("sp", xt[:], Xsrc[i * 128:(i + 1) * 128, :], reads=[xkey(i)], writes=[xk])
            ss, sk = ssr.next()
            rms_rstd(k, xt[:], xk, junk[:], "junk", ss[:], sk, D)
            h, hk = hr.next()
            stt(k, h[:], xt[:], ss[:, 0:1], gt[:], ALU.mult, ALU.mult, [xk, sk, "g"], [hk])
            pt, pk = ptr.next()
            for c in range(8):
                tp(k, pt[:, c, :], h[:, c * 128:(c + 1) * 128], C["identb"][:], [hk, "identb"], [pk], inc=(c == 7))
            cp(k, "act" if i % 2 else "dve", hT[:, :, i * 128:(i + 1) * 128], pt[:], [pk], [hkey(i)])


def phase_mla(nc, k, C, I, S, l, hT):
    sc = 96.0 ** -0.5
    with Ctx(nc, k, "mla%d" % l) as cx:
        wm = cx.sb([128, 8, 416], BF16, "wm")
        k.dma("pool", wm[:], I["w_in"][l, :, O_CQ:O_CQ + 416].rearrange("(c p) n -> p c n", p=128), writes=["wm"])
        wuq = cx.sb([128, 2, 384], BF16, "wuq")
        k.dma("pool", wuq[:], I["w_uq"][l].rearrange("(c p) n -> p c n", p=128), writes=["wuq"])
        wukv = cx.sb([128, 512], BF16, "wukv")
        k.dma("pool", wukv[:], I["w_ukv"][l], writes=["wukv"])
        gq = bcast_load(k, cx, I["q_norm_g"][l], 256, "gq")
        gkv = bcast_load(k, cx, I["kv_norm_g"][l], 128, "gkv")
        cs2 = cx.sb([128, NT, 32], F32, "cs2"); sn2 = cx.sb([128, NT, 32], F32, "sn2")
        cs2q = cx.sb([128, NT, 4, 32], F32, "cs2q"); sn2q = cx.sb([128, NT, 4, 32], F32, "sn2q")
        k.dma("sp", cs2[:], I["rope_cs2"], writes=["cs2"])
        k.dma("sp", sn2[:], I["rope_sn2"], writes=["sn2"])
        k.dma("sp", cs2q[:], I["rope_cs2q"], writes=["cs2q"])
        k.dma("sp", sn2q[:], I["rope_sn2q"], writes=["sn2q"])
        cqnT = cx.sb([128, 2, L], BF16, "cqnT"); ckvnT = cx.sb([128, L], BF16, "ckvnT")
        qT = cx.sb([128, 4, L], BF16, "qT"); kT = cx.sb([128, 4, L], BF16, "kT")
        vaug = cx.sb([128, NT, 4, 65], BF16, "vaug")
        ymla = cx.sb([128, NT, 256], BF16, "ymla")
        k.op("pool", lambda e: e.memset(vaug[:], 1.0), [], ["vaug_init"])
        qar = cx.rot_sb(3, [128, 4, 128], BF16, "qa"); kar = cx.rot_sb(3, [128, 4, 128], BF16, "ka")
        for j_, t_ in enumerate(qar.tiles):
            k.op("pool", lambda e: e.memset(t_[:], 0.0), [], ["mla%dqa#%d" % (l, j_)])
        for j_, t_ in enumerate(kar.tiles):
            k.op("pool", lambda e: e.memset(t_[:], 0.0), [], ["mla%dka#%d" % (l, j_)])
        with Ctx(nc, k, "mlaP%d" % l) as c2:
            pmr = c2.rot_ps(2, [128, 416], F32, "pm")
            ptr = c2.rot_ps(3, [128, 4, 128], BF16, "ptb")
            pqr = c2.rot_ps(1, [128, 384], F32, "pq")
            pkvr = c2.rot_ps(1, [128, 512], F32, "pkv")
            nrr = c2.rot_sb(4, [128, 416], BF16, "nrm")
            ssr = c2.rot_sb(8, [128, 1], F32, "ss")
            junk = c2.sb([128, 256], F32, "junk")
            tmr = c2.rot_sb(8, [128, 4, 32], F32, "tm")
            st = {}

            def stA(i):
                tsl = slice(i * 128, (i + 1) * 128)
                pm, pmk = pmr.next()
                for c in range(8):
                    mm(k, pm[:], hT[:, c, tsl], wm[:, c, :], c == 0, c == 7, ["hT%d" % i, "wm"], [pmk], inc=(c == 7))
                sq, sqk = ssr.next(); skv, skvk = ssr.next()
                rms_rstd(k, pm[:, 0:256], pmk, junk[:, 0:256], "junk", sq[:], sqk, 256)
                rms_rstd(k, pm[:, 256:384], pmk, junk[:, 0:128], "junk", skv[:], skvk, 128)
                nrm, nk = nrr.next()
                stt(k, nrm[:, 0:256], pm[:, 0:256], sq[:, 0:1], gq[:], ALU.mult, ALU.mult, [pmk, sqk, "gq"], [nk])
                stt(k, nrm[:, 256:384], pm[:, 256:384], skv[:, 0:1], gkv[:], ALU.mult, ALU.mult, [pmk, skvk, "gkv"], [nk])
                t1, t1k = tmr.next(); t2, t2k = tmr.next()
                tt(k, "dve", t1[:, 0, :], pm[:, 384:416], cs2[:, i, :], ALU.mult, [pmk, "cs2"], [t1k])
                tt(k, "dve", t2[:, 0, 0:16], pm[:, 400:416], sn2[:, i, 0:16], ALU.mult, [pmk, "sn2"], [t2k])
                tt(k, "dve", t2[:, 0, 16:32], pm[:, 384:400], sn2[:, i, 16:32], ALU.mult, [pmk, "sn2"], [t2k])
                tt(k, "dve", nrm[:, 384:416], t1[:, 0, :], t2[:, 0, :], ALU.add, [t1k, t2k], [nk])
                st[i] = {"nrm": nrm, "nk": nk}

            def stB(i):
                tsl = slice(i * 128, (i + 1) * 128)
                nrm, nk = st[i]["nrm"], st[i]["nk"]
                pt, ptk = ptr.next()
                for c in range(3):
                    tp(k, pt[:, c, :], nrm[:, c * 128:(c + 1) * 128], C["identb"][:], [nk, "identb"], [ptk], inc=(c == 2))
                cp(k, "act", cqnT[:, :, tsl], pt[:, 0:2, :], [ptk], ["cqnT%d" % i])
                cp(k, "act", ckvnT[:, tsl], pt[:, 2, :], [ptk], ["ckvnT%d" % i])

            def stC(i):
                tsl = slice(i * 128, (i + 1) * 128)
                nrm, nk = st[i]["nrm"], st[i]["nk"]
                qa, qak = qar.next(); ka, kak = kar.next()
                pq, pqk = pqr.next()
                for c in range(2):
                    mm(k, pq[:], cqnT[:, c, tsl], wuq[:, c, :], c == 0, c == 1, ["cqnT%d" % i, "wuq"], [pqk], inc=(c == 1))
                pqv = pq.rearrange("p (h d) -> p h d", h=4)
                ts(k, "dve", qa[:, :, 0:64], pqv[:, :, 0:64], sc, None, ALU.mult, None, [pqk], [qak])
                t3, t3k = tmr.next(); t4, t4k = tmr.next()
                tt(k, "dve", t3[:], pqv[:, :, 64:96], cs2q[:, i], ALU.mult, [pqk, "cs2q"], [t3k])
                tt(k, "dve", t4[:, :, 0:16], pqv[:, :, 80:96], sn2q[:, i, :, 0:16], ALU.mult, [pqk, "sn2q"], [t4k])
                tt(k, "dve", t4[:, :, 16:32], pqv[:, :, 64:80], sn2q[:, i, :, 16:32], ALU.mult, [pqk, "sn2q"], [t4k])
                tt(k, "dve", qa[:, :, 64:96], t3[:], t4[:], ALU.add, [t3k, t4k], [qak])
                pkv, pkvk = pkvr.next()
                mm(k, pkv[:], ckvnT[:, tsl], wukv[:], True, True, ["ckvnT%d" % i, "wukv"], [pkvk])
                pkvv = pkv.rearrange("p (h d) -> p h d", h=4)
                cp(k, "act", ka[:, :, 0:64], pkvv[:, :, 0:64], [pkvk], [kak])
                for h in range(4):
                    cp(k, "dve", ka[:, h, 64:96], nrm[:, 384:416], [nk], [kak])
                cp(k, "act", vaug[:, i, :, 0:64], pkvv[:, :, 64:128], [pkvk, "vaug_init"], ["vaug%d" % i])
                st[i].update({"qa": qa, "qak": qak, "ka": ka, "kak": kak})

            def stD(i):
                tsl = slice(i * 128, (i + 1) * 128)
                qa, qak, ka, kak = st[i]["qa"], st[i]["qak"], st[i]["ka"], st[i]["kak"]
                pt2, pt2k = ptr.next()
                for h in range(4):
                    tp(k, pt2[:, h, :], qa[:, h, :], C["identb"][:], [qak, "identb"], [pt2k], inc=(h == 3))
                cp(k, "dve", qT[:, :, tsl], pt2[:, :, :], [pt2k], ["qT%d" % i])
                pt3, pt3k = ptr.next()
                for h in range(4):
                    tp(k, pt3[:, h, :], ka[:, h, :], C["identb"][:], [kak, "identb"], [pt3k], inc=(h == 3))
                cp(k, "act", kT[:, :, tsl], pt3[:, :, :], [pt3k], ["kT%d" % i])
                del st[i]

            for step in range(NT + 3):
                if step < NT:
                    stA(step)
                if 0 <= step - 1 < NT:
                    stB(step - 1)
                if 0 <= step - 2 < NT:
                    stC(step - 2)
                if 0 <= step - 3 < NT:
                    stD(step - 3)
        with Ctx(nc, k, "mlaA%d" % l) as c3:
            psr = c3.rot_ps(4, [128, 512], F32, "s")
            por = c3.rot_ps(2, [128, 4, 128], F32, "o")
            ppr = c3.rot_sb(4, [128, 512], BF16, "pT")
            rcr = c3.rot_sb(2, [128, 4, 1], F32, "rc")
            allq = ["qT%d" % i for i in range(NT)]
            items = [(h, qb, kt) for h in range(4) for qb in range(4) for kt in range(NT)]
            LOOK = 2
            sbuf = {}

            def emit_S(idx):
                h, qb, kt = items[idx]
                ps_, psk = psr.next()
                mm(k, ps_[:], kT[:, h, kt * 128:(kt + 1) * 128], qT[:, h, qb * 512:(qb + 1) * 512], True, True,
                   ["kT%d" % kt] + allq[qb * 4:qb * 4 + 4], [psk])
                sbuf[idx] = (ps_, psk)

            for j in range(min(LOOK, len(items))):
                emit_S(j)
            po = pok = None
            for idx, (h, qb, kt) in enumerate(items):
                if idx + LOOK < len(items):
                    emit_S(idx + LOOK)
                if kt == 0:
                    po, pok = por.next()
                ps_, psk = sbuf.pop(idx)
                pT, pTk = ppr.next()
                act(k, pT[:], ps_[:], AF.Exp, [psk], [pTk])
                for qs in range(4):
                    mm(k, po[:, qs, 0:65], pT[:, qs * 128:(qs + 1) * 128], vaug[:, kt, h, :], kt == 0 and qs == 0, kt == NT - 1 and qs == 3,
                       [pTk, "vaug%d" % kt], [pok], inc=(qs == 3))
                if kt == NT - 1:
                    rc, rck = rcr.next()
                    k.op("dve", lambda e: e.reciprocal(out=rc[:], in_=po[:, :, 64:65]), [pok], [rck])
                    tt(k, "dve", ymla[:, qb * 4:(qb + 1) * 4, h * 64:(h + 1) * 64], po[:, :, 0:64],
                       rc[:].broadcast_to([128, 4, 64]), ALU.mult, [pok, rck], ["ymla%d" % qb])
        with Ctx(nc, k, "mlaT%d" % l) as c4:
            t_to_f(k, c4, C, ymla, lambda i: "ymla%d" % (i // 4), S["Yd"][2], "Yd2")


def phase_na(nc, k, C, I, S, l, hT):
    sc = 64.0 ** -0.5
    with Ctx(nc, k, "na%d" % l) as cx:
        wna = cx.sb([128, 8, 768], BF16, "wna")
        k.dma("pool", wna[:], I["w_in"][l, :, O_NA:O_NA + 768].rearrange("(c p) n -> p c n", p=128), writes=["wna"])
        qT = cx.sb([128, 2, L], BF16, "qT"); kT = cx.sb([128, 2, L], BF16, "kT")
        va0 = cx.sb([128, NT, 4, 65], BF16, "va0"); va1 = cx.sb([128, NT, 4, 65], BF16, "va1")
        yna = cx.sb([128, NT, 256], BF16, "yna")
        T2 = cx.sb([128, 4, 14, 64], BF16, "T2")
        k.op("pool", lambda e: e.memset(va0[:], 1.0), [], ["va0i"])
        k.op("pool", lambda e: e.memset(va1[:], 1.0), [], ["va1i"])
        with Ctx(nc, k, "naB%d" % l) as cb:
            rp = cb.sb([32, 60], F32, "rp")
            k.op("pool", lambda e: e.memset(rp[:], 1.0), [], ["rp"])
            k.dma("sp", rp[0:31, :], I["rpb"][l].rearrange("h r c -> c (h r)"), reads=["rp"], writes=["rp"], allow_slow_non_contiguous=True)
            E = cb.sb([32, 4096], F32, "E")
            k.dma("sp", E[:], I["na_E"], writes=["E"])
            bsb = cb.sb([60, 4096], F32, "bsb")
            pbr = cb.rot_ps(2, [128, 512], F32, "pb")
            for j in range(8):
                pb, pbk = pbr.next()
                mm(k, pb[0:60, :], rp[:, :], E[:, j * 512:(j + 1) * 512], True, True, ["rp", "E"], [pbk])
                cp(k, "act" if j % 2 else "dve", bsb[:, j * 512:(j + 1) * 512], pb[0:60, :], [pbk], ["bsb"])
            k.dma("sp", S["bias_d"], bsb[:], reads=["bsb"], writes=["bias_d"])
            T2f = cb.sb([128, 4, 14, 64], F32, "T2f")
            bv = S["bias_d"].rearrange("(h r) (kc qc) -> kc h r qc", h=4, kc=64)
            for h in range(4):
                k.dma("sp", T2f[0:64, h], bv[:, h, 0:14, :], reads=["bias_d"], writes=["T2f"])
                k.dma("sp", T2f[64:128, h], bv[:, h, 1:15, :], reads=["bias_d"], writes=["T2f"])
            cp(k, "dve", T2[:], T2f[:], ["T2f"], ["T2"])
        with Ctx(nc, k, "naP%d" % l) as c2:
            psr = c2.rot_ps(3, [128, 512], F32, "ps")
            for cc in range(4):
                for tb in range(4):
                    ps_, psk = psr.next()
                    hk = ["hT%d" % (tb * 4 + j) for j in range(4)]
                    for c in range(8):
                        mm(k, ps_[:], wna[:, c, cc * 128:(cc + 1) * 128], hT[:, c, tb * 512:(tb + 1) * 512], c == 0, c == 7, hk + ["wna"], [psk], inc=(c == 7))
                    if cc < 2:
                        k.op("act", lambda e: e.mul(qT[:, cc, tb * 512:(tb + 1) * 512], ps_[:], sc), [psk], ["qT%d_%d" % (cc, tb)])
                    else:
                        cp(k, "dve", kT[:, cc - 2, tb * 512:(tb + 1) * 512], ps_[:], [psk], ["kT%d_%d" % (cc - 2, tb)])
            for i in range(NT):
                ps_, psk = psr.next()
                for c in range(8):
                    mm(k, ps_[:, 0:256], hT[:, c, i * 128:(i + 1) * 128], wna[:, c, 512:768], c == 0, c == 7, ["hT%d" % i, "wna"], [psk], inc=(c == 7))
                cp(k, "act" if i % 2 else "dve", va0[:, i, :, 0:64], ps_[:, 0:256].rearrange("p (h d) -> p h d", h=4), [psk, "va0i"], ["va0_%d" % i])
            for i in range(NT - 1):
                ps_, psk = psr.next()
                for c in range(8):
                    mm(k, ps_[:, 0:256], hT[:, c, 64 + i * 128:64 + (i + 1) * 128], wna[:, c, 512:768], c == 0, c == 7, ["hT%d" % i, "hT%d" % (i + 1), "wna"], [psk], inc=(c == 7))
                cp(k, "act" if i % 2 else "dve", va1[:, i, :, 0:64], ps_[:, 0:256].rearrange("p (h d) -> p h d", h=4), [psk, "va1i"], ["va1_%d" % i])
        with Ctx(nc, k, "naA%d" % l) as c3:
            psr = c3.rot_ps(4, [128, 4, 64], F32, "s")
            por = c3.rot_ps(2, [128, 4, 128], F32, "o")
            ppr = c3.rot_sb(4, [128, 4, 64], BF16, "pT")
            rcr = c3.rot_sb(2, [128, 4, 1], F32, "rc")
            items = [(r, h) for r in range(32) for h in range(4)]
            LOOK = 2
            sbuf = {}

            def emit_S(idx):
                r, h = items[idx]
                r0 = min(max(r - 4, 0), 24)
                ch, hp = h // 2, (h % 2) * 64
                ps_, psk = psr.next()
                for j in range(4):
                    ktok = (r0 + 2 * j) * 64
                    dr1 = r0 + 2 * j - r + 7
                    kkeys = ["kT%d_%d" % (ch, tbb) for tbb in sorted(set([ktok // 512, (ktok + 127) // 512]))]
                    mm(k, ps_[:, j, :], kT[hp:hp + 64, ch, ktok:ktok + 128], qT[hp:hp + 64, ch, r * 64:(r + 1) * 64], True, False,
                       kkeys + ["qT%d_%d" % (ch, r // 8)], [psk], inc=False)
                    mm(k, ps_[:, j, :], C["identb"][:], T2[:, h, dr1, :], False, True, ["identb", "T2"], [psk], inc=(j == 3))
                sbuf[idx] = (ps_, psk)

            for j in range(LOOK):
                emit_S(j)
            po = pok = None
            for idx, (r, h) in enumerate(items):
                if idx + LOOK < len(items):
                    emit_S(idx + LOOK)
                r0 = min(max(r - 4, 0), 24)
                if r % 2 == 0 and h == 0:
                    po, pok = por.next()
                ro = (r % 2) * 64
                ps_, psk = sbuf.pop(idx)
                pT, pTk = ppr.next()
                act(k, pT[:], ps_[:], AF.Exp, [psk], [pTk])
                for j in range(4):
                    rr = r0 + 2 * j
                    if rr % 2 == 0:
                        vs, vkey = va0[:, rr // 2, h, :], "va0_%d" % (rr // 2)
                    else:
                        vs, vkey = va1[:, (rr - 1) // 2, h, :], "va1_%d" % ((rr - 1) // 2)
                    mm(k, po[ro:ro + 64, h, 0:65], pT[:, j, :], vs, j == 0, j == 3, [pTk, vkey], [pok], inc=(j == 3))
                if r % 2 == 1 and h == 3:
                    ti = r // 2
                    rc, rck = rcr.next()
                    k.op("dve", lambda e: e.reciprocal(out=rc[:], in_=po[:, :, 64:65]), [pok], [rck])
                    tt(k, "dve", yna[:, ti, :].rearrange("p (h d) -> p h d", h=4), po[:, :, 0:64],
                       rc[:].broadcast_to([128, 4, 64]), ALU.mult, [pok, rck], ["yna%d" % ti])
        with Ctx(nc, k, "naT%d" % l) as c4:
            t_to_f(k, c4, C, yna, lambda i: "yna%d" % i, S["Yd"][3], "Yd3")


def phase_hyfn_prep(nc, k, C, I, S, l, hT):
    import os as _os
    PLV = int(_os.environ.get("PREP_LV", "9"))
    with Ctx(nc, k, "hp%d" % l) as cx:
        wh = cx.sb([128, 8, 1024], BF16, "wh")
        for j in range(2):
            k.dma("pool", wh[:, :, j * 512:(j + 1) * 512], I["w_in"][l, :, j * 512:(j + 1) * 512].rearrange("(c p) n -> p c n", p=128), writes=["wh%d" % j])
        whk = ["wh0", "wh1"]
        cw = cx.sb([128, 6, 3], F32, "cw")
        for kk_ in range(3):
            k.dma("sp", cw[:, :, kk_], I["hy_conv_w"][l, kk_].rearrange("(cc p) -> p cc", p=128), writes=["cw"], allow_slow_non_contiguous=True)
        cbias = cx.sb([128, 6], F32, "cb")
        k.dma("sp", cbias[:], I["hy_conv_b"][l].rearrange("(cc p) -> p cc", p=128), writes=["cb"], allow_slow_non_contiguous=True)
        upr = cx.rot_sb(2, [128, L + 2], F32, "up")
        for t_, kk_ in zip(upr.tiles, ["hp%dup#0" % l, "hp%dup#1" % l]):
            k.op("pool", lambda e: e.memset(t_[:, 0:1], 0.0), [], [kk_])
            k.op("pool", lambda e: e.memset(t_[:, L + 1:L + 2], 0.0), [], [kk_])
        ucr = cx.rot_sb(2, [128, L], F32, "uc")
        vbr = cx.rot_sb(2, [128, L], BF16, "vb")
        vst = cx.sb([128, NT, 256], BF16, "vst")
        psr = cx.rot_ps(3, [128, 512], F32, "ps")
        ptr = cx.rot_ps(2, [128, 4, 128], BF16, "pt")
        dsts = [S["x1Td"], S["x2Td"], S["vTd"]]
        for cc in range(6):
            up, upk = upr.next()
            for tb in range(4):
                ps_, psk = psr.next()
                hk = ["hT%d" % (tb * 4 + j) for j in range(4)]
                for c in range(8):
                    mm(k, ps_[:], wh[:, c, cc * 128:(cc + 1) * 128], hT[:, c, tb * 512:(tb + 1) * 512], c == 0, c == 7, hk + whk, [psk], inc=(c == 7))
                cp(k, "act" if tb % 2 else "dve", up[:, 1 + tb * 512:1 + (tb + 1) * 512], ps_[:], [psk], [upk])
            if PLV <= 1:
                continue
            u_c, uck = ucr.next()
            ts(k, "dve", u_c[:], up[:, 1:L + 1], cw[:, cc, 1:2], cbias[:, cc:cc + 1], ALU.mult, ALU.add, [upk, "cw", "cb"], [uck])
            stt(k, u_c[:], up[:, 0:L], cw[:, cc, 0:1], u_c[:], ALU.mult, ALU.add, [upk, "cw", uck], [uck])
            stt(k, u_c[:], up[:, 2:L + 2], cw[:, cc, 2:3], u_c[:], ALU.mult, ALU.add, [upk, "cw", uck], [uck])
            dst = dsts[cc // 2][(cc % 2) * 128:(cc % 2) * 128 + 128, :]
            k.dma("sp", dst, u_c[:], reads=[uck], writes=["hyd%d" % cc])
            if PLV <= 2:
                continue
            if cc >= 4:
                vb, vbk = vbr.next()
                cp(k, "pool", vb[:], u_c[:], [uck], [vbk])
                for g in range(4):
                    pt, ptk = ptr.next()
                    for j in range(4):
                        i = g * 4 + j
                        tp(k, pt[:, j, :], vb[:, i * 128:(i + 1) * 128], C["identb"][:], [vbk, "identb"], [ptk], inc=(j == 3))
                    cp(k, "act" if g % 2 else "dve", vst[:, g * 4:(g + 1) * 4, (cc - 4) * 128:(cc - 3) * 128], pt[:], [ptk], ["vst%d" % (cc - 4)])
        if PLV <= 3:
            return
        k.dma("sp", S["v_d"].rearrange("(c p) n -> p c n", p=128), vst[:], reads=["vst0", "vst1"], writes=["v_d"])
        if PLV <= 4:
            return
        PX = _os.environ.get("PREPX", "")
        sgn = cx.sb([128, 1], F32, "sgn")
        if "nodma" in PX:
            k.op("dve", lambda e: e.memset(sgn[:], 1.0), [], ["sgn"])
        else:
            k.dma("sp", sgn[:], I["signp"], writes=["sgn"])
        uf0 = cx.sb([128, NT, 256], BF16, "uf0"); uf1 = cx.sb([128, NT, 256], BF16, "uf1")
        for i in range(8 if "half" in PX else NT):
            ps_, psk = psr.next()
            for c in range(8):
                mm(k, ps_[:, 0:256], hT[:, c, i * 128:(i + 1) * 128], wh[:, c, 768:1024], c == 0, c == 7, ["hT%d" % i] + whk, [psk], inc=(c == 7))
            if "noact" not in PX:
                cp(k, "act", uf0[:, i, :], ps_[:, 0:256], [psk], ["uf0"])
            if "nodve" not in PX:
                ts(k, "dve", uf1[:, i, :], ps_[:, 0:256], sgn[:, 0:1], None, ALU.mult, None, [psk, "sgn"], ["uf1"])
        if "nodma" in PX:
            return
        k.dma("sp", S["fn_d"][0].rearrange("(c p) n -> p c n", p=128), uf0[:], reads=["uf0"], writes=["fn_d0"])
        k.dma("sp", S["fn_d"][1].rearrange("(c p) n -> p c n", p=128), uf1[:], reads=["uf1"], writes=["fn_d1"])


def phase_dft(nc, k, C, I, S, l):
    TWO_PI = 2.0 * math.pi
    import os as _os
    HLV = int(_os.environ.get("HY_LV", "9"))
    if HLV <= 1:
        return
    with Ctx(nc, k, "dft%d" % l) as cx:
        A = cx.sb([128, NT, L], BF16, "A"); B = cx.sb([128, NT, L], BF16, "B")
        Av = I["dftA"].rearrange("(c p) f -> p c f", p=128); Bv = I["dftB"].rearrange("(c p) f -> p c f", p=128)
        for j in range(4):
            k.dma("sp", A[:, j * 4:(j + 1) * 4, :], Av[:, j * 4:(j + 1) * 4, :], writes=["A%d" % j])
            k.dma("act", B[:, j * 4:(j + 1) * 4, :], Bv[:, j * 4:(j + 1) * 4, :], writes=["B%d" % j])
        AK = ["A%d" % j for j in range(4)]; BK = ["B%d" % j for j in range(4)]
        altcol = cx.sb([128, 1], BF16, "altcol"); altrow = cx.sb([1, L], BF16, "altrow")
        k.dma("pool", altcol[:], I["altcol"], writes=["altcol"])
        k.dma("pool", altrow[:], I["altrow"], writes=["altrow"])
        sk = cx.sb([128, 2, 2], F32, "skip")
        for o_ in range(2):
            k.dma("sp", sk[:, o_, :], I["hy_skip"][l, o_].rearrange("(cc p) -> p cc", p=128), writes=["skip"], allow_slow_non_contiguous=True)
        if HLV <= 2:
            return
        with Ctx(nc, k, "flt%d" % l) as cf:
            hf2T = cf.sb([64, L], F32, "hf2T")
            w3 = cf.sb([64, 1024], F32, "w3")
            k.dma("sp", w3[:], I["hf_w3"][l], writes=["w3"])
            onesf = cf.sb([128, 128], F32, "onesf")
            k.dma("sp", onesf[:], I["ones_f"], writes=["onesf"])
            with Ctx(nc, k, "mlp%d" % l) as cm:
                zT = cm.sb([33, L], F32, "zT")
                k.dma("sp", zT[:], I["hy_zT"], writes=["zT"])
                w1 = cm.sb([33, 64], F32, "w1"); w2 = cm.sb([64, 64], F32, "w2")
                k.dma("sp", w1[:], I["hf_w1"][l], writes=["w1"])
                k.dma("sp", w2[:], I["hf_w2"][l], writes=["w2"])
                bb = cm.sb([64, 2], F32, "bb"); fr = cm.sb([64, 2], F32, "fr")
                k.dma("sp", bb[:, 0:1], I["hf_b1"][l].rearrange("(p o) -> p o", o=1), writes=["bb"])
                k.dma("sp", bb[:, 1:2], I["hf_b2"][l].rearrange("(p o) -> p o", o=1), writes=["bb"])
                k.dma("sp", fr[:], I["hf_freq"][l].rearrange("k p -> p k"), writes=["fr"], allow_slow_non_contiguous=True)
                fb = cm.sb([64, 2], F32, "fb")
                tt(k, "dve", fb[:], fr[:], bb[:], ALU.mult, ["fr", "bb"], ["fb"])
                negpi = cm.sb([64, 1], F32, "negpi")
                k.op("pool", lambda e: e.memset(negpi[:], -math.pi), [], ["negpi"])
                hf1T = cm.sb([64, L], F32, "hf1T"); arg = cm.sb([64, L], F32, "arg"); nn = cm.sb([64, L], F32, "nn")
                pmr = cm.rot_ps(2, [128, 512], F32, "pm")
                for (W, src, srck, dst, dstk, j, kdim) in ((w1, zT, "zT", hf1T, "hf1T", 0, 33), (w2, hf1T, "hf1T", hf2T, "hf2T", 1, 64)):
                    for tb in range(4):
                        tsl = slice(tb * 512, (tb + 1) * 512)
                        pm, pmk = pmr.next()
                        mm(k, pm[0:64, :], W[0:kdim, :], src[0:kdim, tsl], True, True, ["w1", "w2", "zT" if j == 0 else "hf1T%d" % tb], [pmk])
                        ts(k, "dve", arg[:, tsl], pm[0:64, :], fr[:, j:j + 1], fb[:, j:j + 1], ALU.mult, ALU.add, [pmk, "fr", "fb"], ["arg%d" % tb])
                        MAGIC = 12582912.0
                        ts(k, "dve", nn[:, tsl], arg[:, tsl], 1.0 / TWO_PI, MAGIC, ALU.mult, ALU.add, ["arg%d" % tb], ["nn%d" % tb])
                        ts(k, "dve", nn[:, tsl], nn[:, tsl], -MAGIC, None, ALU.add, None, ["nn%d" % tb], ["nn%d" % tb])
                        stt(k, arg[:, tsl], nn[:, tsl], -TWO_PI, arg[:, tsl], ALU.mult, ALU.add, ["nn%d" % tb, "arg%d" % tb], ["arg%d" % tb])
                        ts(k, "dve", arg[:, tsl], arg[:, tsl], -math.pi, math.pi, ALU.max, ALU.min, ["arg%d" % tb], ["arg%d" % tb])
                        act(k, dst[:, tsl], arg[:, tsl], AF.Sin, ["arg%d" % tb], [dstk + str(tb)])
            if HLV <= 3:
                return
            kkr = cf.rot_sb(3, [128, 512], F32, "kk")
            P = cf.sb([128, NT, 256], BF16, "P"); Q = cf.sb([128, NT, 256], BF16, "Q")
            wtr = cf.rot_sb(2, [128, 256], F32, "wt")
            sqr = cf.rot_sb(2, [128, 512], F32, "sq")
            tmr = cf.rot_sb(2, [128, 256], F32, "tm")
            kor = cf.rot_sb(4, [128, 256], F32, "ko")
            pss_sb = cf.sb([128, 512], F32, "pss_sb")
            rn = cf.sb([128, 256], F32, "rn")
            kn = cf.sb([1, 256], F32, "kn")
            p3r = cf.rot_ps(2, [128, 512], F32, "p3")
            pssr = cf.rot_ps(1, [128, 512], F32, "pss")
            par = cf.rot_ps(2, [128, 256], F32, "pa")
            pbr = cf.rot_ps(2, [128, 256], F32, "pb")
            pnr = cf.rot_ps(1, [128, 256], F32, "pn")
            hfk = ["hf2T%d" % tb for tb in range(4)]

            def kk_tile(o, i):
                p3, p3k = p3r.next()
                mm(k, p3[:], hf2T[:, i * 128:(i + 1) * 128], w3[:, o * 512:(o + 1) * 512], True, True, [hfk[i // 4], "w3"], [p3k])
                wt, wtk = wtr.next()
                k.dma("sp", wt[:], I["hy_win"][i * 128:(i + 1) * 128, :], writes=[wtk])
                kt_, ktk = kkr.next()
                tt(k, "dve", kt_[:].rearrange("p (a c) -> p a c", a=2), p3[:].rearrange("p (a c) -> p a c", a=2),
                   wt[:].unsqueeze(1).broadcast_to([128, 2, 256]), ALU.mult, [p3k, wtk], [ktk])
                if i == 0:
                    k.op("pool", lambda e: e.memset(kt_[0:1, 256:512], 0.0), [ktk], [ktk])
                return kt_, ktk

            for o in range(2):
                pss, pssk = pssr.next()
                for i in range(NT):
                    kt_, ktk = kk_tile(o, i)
                    sq, sqk = sqr.next()
                    act(k, sq[:], kt_[:], AF.Square, [ktk], [sqk])
                    mm(k, pss[:], onesf[:], sq[:], i == 0, i == NT - 1, ["onesf", sqk], [pssk], inc=True)
                cp(k, "act", pss_sb[:], pss[:], [pssk], ["pss_sb"])
                tt(k, "dve", rn[:], pss_sb[:, 0:256], pss_sb[:, 256:512], ALU.add, ["pss_sb"], ["rn"])
                ts(k, "dve", rn[:], rn[:], EPS, None, ALU.add, None, ["rn"], ["rn"])
                act(k, rn[:], rn[:], AF.Sqrt, ["rn"], ["rn"])
                k.op("dve", lambda e: e.reciprocal(out=rn[:], in_=rn[:]), ["rn"], ["rn"])
                for i in range(NT):
                    kt_, ktk = kk_tile(o, i)
                    tm, tmk = tmr.next()
                    tt(k, "pool", tm[:], kt_[:, 0:256], kt_[:, 256:512], ALU.add, [ktk], [tmk])
                    tt(k, "dve", P[:, i, :], tm[:], rn[:], ALU.mult, [tmk, "rn"], ["P%d" % i])
                    tm2, tm2k = tmr.next()
                    tt(k, "pool", tm2[:], kt_[:, 0:256], kt_[:, 256:512], ALU.subtract, [ktk], [tm2k])
                    tt(k, "dve", Q[:, i, :], tm2[:], rn[:], ALU.mult, [tm2k, "rn"], ["Q%d" % i])
                PK = ["P%d" % i for i in range(NT)]; QK = ["Q%d" % i for i in range(NT)]
                for fc in range(NT):
                    pa, pak = par.next(); pb, pbk = pbr.next()
                    for tc in range(NT):
                        mm(k, pa[:], A[:, tc, fc * 128:(fc + 1) * 128], P[:, tc, :], tc == 0, tc == NT - 1, [AK[tc // 4], PK[tc]], [pak], inc=(tc == NT - 1))
                    for tc in range(NT):
                        mm(k, pb[:], B[:, tc, fc * 128:(fc + 1) * 128], Q[:, tc, :], tc == 0, tc == NT - 1, [BK[tc // 4], QK[tc]], [pbk], inc=(tc == NT - 1))
                    ka, kak = kor.next(); kb, kbk = kor.next()
                    act(k, ka[:], pa[:], AF.Copy, [pak], [kak], scale=2.0 / NFFT)
                    ts(k, "dve", kb[:], pb[:], 2.0 / NFFT, None, ALU.mult, None, [pbk], [kbk])
                    if fc == 0:
                        ts(k, "dve", ka[0:1, :], ka[0:1, :], 0.5, None, ALU.mult, None, [kak], [kak])
                    k.dma("sp", S["KAd"][o, fc * 128:(fc + 1) * 128, :], ka[:], reads=[kak], writes=["KAd%d_%d" % (o, fc)])
                    k.dma("sp", S["KBd"][o, fc * 128:(fc + 1) * 128, :], kb[:], reads=[kbk], writes=["KBd%d_%d" % (o, fc)])
                pn, pnk = pnr.next()
                for tc in range(NT):
                    mm(k, pn[0:1, :], altcol[:, 0:1], P[:, tc, :], tc == 0, tc == NT - 1, ["altcol", PK[tc]], [pnk], inc=(tc == NT - 1))
                ts(k, "dve", kn[:], pn[0:1, :], 1.0 / NFFT, None, ALU.mult, None, [pnk], ["kn"])
                k.dma("sp", S["KNd"][o:o + 1, :], kn[:], reads=["kn"], writes=["KNd%d" % o])
        if HLV <= 4:
            return
        for o in range(2):
            if HLV <= 5 and o == 1:
                break
            with Ctx(nc, k, "cv%d_%d" % (l, o)) as cc:
                z = cc.sb([128, NT, 256], BF16, "z")
                k.dma("sp", z[:], (S["v_d"] if o == 0 else S["z2_d"]).rearrange("(c p) n -> p c n", p=128),
                      reads=["v_d" if o == 0 else "z2_d"], writes=["z"])
                YA = cc.sb([128, NT, 256], BF16, "YA"); YB = cc.sb([128, NT, 256], BF16, "YB")
                kn = cc.sb([1, 256], F32, "kn")
                k.dma("sp", kn[:], S["KNd"][o:o + 1, :], reads=["KNd%d" % o], writes=["kn"])
                yn = cc.sb([1, 256], BF16, "yn")
                with Ctx(nc, k, "cvf%d_%d" % (l, o)) as c1:
                    par = c1.rot_ps(2, [128, 256], F32, "pa"); pbr = c1.rot_ps(2, [128, 256], F32, "pb")
                    pnr = c1.rot_ps(1, [128, 256], F32, "pn")
                    kar = c1.rot_sb(3, [128, 256], F32, "ka"); kbr = c1.rot_sb(3, [128, 256], F32, "kb")
                    tmr = c1.rot_sb(8, [128, 256], F32, "tm")
                    for fc in range(NT):
                        pa, pak = par.next(); pb, pbk = pbr.next()
                        for tc in range(NT):
                            mm(k, pa[:], A[:, tc, fc * 128:(fc + 1) * 128], z[:, tc, :], tc == 0, tc == NT - 1, [AK[tc // 4], "z"], [pak], inc=(tc == NT - 1))
                        for tc in range(NT):
                            mm(k, pb[:], B[:, tc, fc * 128:(fc + 1) * 128], z[:, tc, :], tc == 0, tc == NT - 1, [BK[tc // 4], "z"], [pbk], inc=(tc == NT - 1))
                        ka, kak = kar.next(); kb, kbk = kbr.next()
                        k.dma("sp", ka[:], S["KAd"][o, fc * 128:(fc + 1) * 128, :], reads=["KAd%d_%d" % (o, fc)], writes=[kak])
                        k.dma("sp", kb[:], S["KBd"][o, fc * 128:(fc + 1) * 128, :], reads=["KBd%d_%d" % (o, fc)], writes=[kbk])
                        t1, t1k = tmr.next(); t2, t2k = tmr.next(); t3, t3k = tmr.next(); t4, t4k = tmr.next()
                        tt(k, "dve", t1[:], pa[:], ka[:], ALU.mult, [pak, kak], [t1k])
                        tt(k, "dve", t2[:], pb[:], kb[:], ALU.mult, [pbk, kbk], [t2k])
                        tt(k, "dve", t3[:], pa[:], kb[:], ALU.mult, [pak, kbk], [t3k])
                        tt(k, "dve", t4[:], pb[:], ka[:], ALU.mult, [pbk, kak], [t4k])
                        tt(k, "pool", YA[:, fc, :], t1[:], t2[:], ALU.subtract, [t1k, t2k], ["YA%d" % fc])
                        tt(k, "pool", YB[:, fc, :], t3[:], t4[:], ALU.add, [t3k, t4k], ["YB%d" % fc])
                    pn, pnk = pnr.next()
                    for tc in range(NT):
                        mm(k, pn[0:1, :], altcol[:, 0:1], z[:, tc, :], tc == 0, tc == NT - 1, ["altcol", "z"], [pnk], inc=(tc == NT - 1))
                    tt(k, "dve", yn[:], pn[0:1, :], kn[:], ALU.mult, [pnk, "kn"], ["yn"])
                with Ctx(nc, k, "cvi%d_%d" % (l, o)) as c2:
                    pyr = c2.rot_ps(2, [128, 512], F32, "py")
                    ptr = c2.rot_ps(2, [128, 4, 128], BF16, "pt")
                    xr = c2.rot_sb(2, [128, 512], F32, "xm"); sr = c2.rot_sb(2, [128, 512], F32, "sm")
                    tmr = c2.rot_sb(2, [128, 512], F32, "tm"); zr = c2.rot_sb(2, [128, 512], F32, "zo")
                    zbr = c2.rot_sb(2, [128, 512], BF16, "zb")
                    if o == 0:
                        z2st = c2.sb([128, NT, 256], BF16, "z2st")
                    else:
                        yst = c2.sb([128, 2, L], BF16, "yst")
                    YAK = ["YA%d" % fc for fc in range(NT)]; YBK = ["YB%d" % fc for fc in range(NT)]
                    xsrc = S["x1Td"] if o == 0 else S["x2Td"]
                    ssrc = S["vTd"] if o == 0 else S["z2Td"]
                    for cch in range(2):
                        csl = slice(cch * 128, (cch + 1) * 128)
                        for tb in range(4):
                            tsl = slice(tb * 512, (tb + 1) * 512)
                            py, pyk = pyr.next()
                            for fc in range(NT):
                                mm(k, py[:], YA[:, fc, csl], A[:, fc, tsl], fc == 0, False, [YAK[fc], AK[fc // 4]], [pyk], inc=False)
                            for fc in range(NT):
                                mm(k, py[:], YB[:, fc, csl], B[:, fc, tsl], False, False, [YBK[fc], BK[fc // 4]], [pyk], inc=False)
                            mm(k, py[:], yn[0:1, csl], altrow[0:1, tsl], False, True, ["yn", "altrow"], [pyk], inc=True)
                            xm, xmk = xr.next(); sm, smk = sr.next()
                            skeys = (["hyd%d" % (4 + cch)] if o == 0 else ["z2Td%d_%d" % (cch, tb)])
                            k.dma("sp", xm[:], xsrc[csl, tsl], reads=["hyd%d" % ((0 if o == 0 else 2) + cch)], writes=[xmk])
                            k.dma("sp", sm[:], ssrc[csl, tsl], reads=skeys, writes=[smk])
                            tm, tmk = tmr.next()
                            stt(k, tm[:], sm[:], sk[:, o, cch:cch + 1], py[:], ALU.mult, ALU.add, [smk, "skip", pyk], [tmk])
                            if o == 0:
                                zo, zok = zr.next()
                                tt(k, "dve", zo[:], tm[:], xm[:], ALU.mult, [tmk, xmk], [zok])
                                k.dma("sp", S["z2Td"][csl, tsl], zo[:], reads=[zok], writes=["z2Td%d_%d" % (cch, tb)])
                                zb, zbk = zbr.next()
                                cp(k, "act", zb[:], zo[:], [zok], [zbk])
                                pt, ptk = ptr.next()
                                for j in range(4):
                                    tp(k, pt[:, j, :], zb[:, j * 128:(j + 1) * 128], C["identb"][:], [zbk, "identb"], [ptk], inc=(j == 3))
                                cp(k, "act", z2st[:, tb * 4:(tb + 1) * 4, csl], pt[:], [ptk], ["z2st"])
                            else:
                                tt(k, "dve", yst[:, cch, tsl], tm[:], xm[:], ALU.mult, [tmk, xmk], ["yst"])
                    if o == 0:
                        k.dma("sp", S["z2_d"].rearrange("(c p) n -> p c n", p=128), z2st[:], reads=["z2st"], writes=["z2_d"])
                    else:
                        k.dma("sp", S["Yd"][0].rearrange("(c p) t -> p c t", p=128), yst[:], reads=["yst"], writes=["Yd0"])
        if HLV <= 6:
            return
        with Ctx(nc, k, "fn%d" % l) as cf:
            uf0 = cf.sb([128, NT, 256], BF16, "uf0"); uf1 = cf.sb([128, NT, 256], BF16, "uf1")
            k.dma("sp", uf0[:], S["fn_d"][0].rearrange("(c p) n -> p c n", p=128), reads=["fn_d0"], writes=["uf0"])
            k.dma("sp", uf1[:], S["fn_d"][1].rearrange("(c p) n -> p c n", p=128), reads=["fn_d1"], writes=["uf1"])
            fC = cf.sb([128, 2, 256], BF16, "fC"); fS = cf.sb([128, 2, 256], BF16, "fS")
            k.dma("pool", fC[:], I["fn_C"].rearrange("(c p) n -> p c n", p=128), writes=["fC"])
            k.dma("pool", fS[:], I["fn_S"].rearrange("(c p) n -> p c n", p=128), writes=["fS"])
            VT = cf.sb([128, 2, 2, L], BF16, "VT")
            yst = cf.sb([128, 2, L], BF16, "yst")
            pyr = cf.rot_ps(3, [128, 512], F32, "py")
            for cs, (M_, MK) in enumerate(((A, AK), (B, BK))):
                for cch in range(2):
                    for lb in range(4):
                        src, srck = (uf0, "uf0") if lb < 2 else (uf1, "uf1")
                        base = (lb % 2) * 1024
                        py, pyk = pyr.next()
                        for lc in range(NT):
                            mm(k, py[:], src[:, lc, cch * 128:(cch + 1) * 128], M_[:, lc, base:base + 1024:2], lc == 0, lc == NT - 1, [srck, MK[lc // 4]], [pyk], inc=(lc == NT - 1))
                        cp(k, "act" if lb % 2 else "dve", VT[:, cs, cch, lb * 512:(lb + 1) * 512], py[:], [pyk], ["VT%d_%d_%d" % (cs, cch, lb)])
            for cch in range(2):
                for lb in range(4):
                    py, pyk = pyr.next()
                    mm(k, py[:], fC[:, cch, cch * 128:(cch + 1) * 128], VT[:, 0, cch, lb * 512:(lb + 1) * 512], True, False, ["fC", "VT0_%d_%d" % (cch, lb)], [pyk], inc=False)
                    mm(k, py[:], fS[:, cch, cch * 128:(cch + 1) * 128], VT[:, 1, cch, lb * 512:(lb + 1) * 512], False, True, ["fS", "VT1_%d_%d" % (cch, lb)], [pyk], inc=True)
                    cp(k, "act" if lb % 2 else "dve", yst[:, cch, lb * 512:(lb + 1) * 512], py[:], [pyk], ["yst"])
            k.dma("sp", S["Yd"][1].rearrange("(c p) t -> p c t", p=128), yst[:], reads=["yst"], writes=["Yd1"])


def phase_gate(nc, k, C, I, S, l):
    with Ctx(nc, k, "g%d" % l) as cx:
        hT = cx.sb([128, 8, L], BF16, "hT")
        for tb in range(4):
            k.dma("sp", hT[:, :, tb * 512:(tb + 1) * 512], S["hTd"][:, :, tb * 512:(tb + 1) * 512], reads=["hTd"], writes=["hT%d" % tb])
        Y = cx.sb([128, 4, 2, L], BF16, "Y")
        for n in range(4):
            k.dma("act", Y[:, n], S["Yd"][n].rearrange("(c p) t -> p c t", p=128), reads=["Yd%d" % n], writes=["Y%d" % n])
        mT = cx.sb([128, 8, L], BF16, "mT")
        wout = cx.sb([128, 8, D], BF16, "wout")
        bg = cx.sb([128, 4, 8], F32, "bg")
        for n in range(4):
            k.dma("sp", bg[:, n, :], I["b_gate"][l, n * 1024:(n + 1) * 1024].rearrange("(dc p) -> p dc", p=128), writes=["bg"], allow_slow_non_contiguous=True)
        wgr = cx.rot_sb(2, [128, 4, 8, 128], BF16, "wg")
        wbr = cx.rot_sb(2, [128, 4, 2, 128], BF16, "wb")
        sgr = cx.rot_sb(3, [128, 512], F32, "sg")
        acr = cx.rot_sb(2, [128, 512], F32, "acc")
        tmr = cx.rot_sb(3, [128, 512], F32, "tmp")
        xr = cx.rot_sb(2, [128, D], F32, "x")
        pgr = cx.rot_ps(3, [128, 512], F32, "pg")
        ppr = cx.rot_ps(3, [128, 512], F32, "pp")
        por = cx.rot_ps(2, [128, 512], F32, "po")
        def load_gw(dc):
            wg, wgk = wgr.next(); wb, wbk = wbr.next()
            for n in range(4):
                col = O_GATE + n * 1024 + dc * 128
                k.dma("pool", wg[:, n], I["w_in"][l, :, col:col + 128].rearrange("(c p) m -> p c m", p=128), writes=[wgk])
                k.dma("pool", wb[:, n], I["w_br"][l, n, :, dc * 128:(dc + 1) * 128].rearrange("(c p) m -> p c m", p=128), writes=[wbk])
            return wg, wgk, wb, wbk

        nxt = load_gw(0)
        for j in range(2):
            k.dma("pool", wout[:, :, j * 512:(j + 1) * 512], I["w_out"][l, :, j * 512:(j + 1) * 512].rearrange("(c p) n -> p c n", p=128), writes=["wout"])
        for dc in range(8):
            wg, wgk, wb, wbk = nxt
            if dc + 1 < 8:
                nxt = load_gw(dc + 1)
            for tb in range(4):
                tsl = slice(tb * 512, (tb + 1) * 512)
                acc, ack = acr.next()
                for n in range(4):
                    pg, pgk = pgr.next(); pp, ppk = ppr.next()
                    for c in range(8):
                        mm(k, pg[:], wg[:, n, c, :], hT[:, c, tsl], c == 0, c == 7, [wgk, "hT%d" % tb], [pgk], inc=(c == 7))
                    for c in range(2):
                        mm(k, pp[:], wb[:, n, c, :], Y[:, n, c, tsl], c == 0, c == 1, [wbk, "Y%d" % n], [ppk], inc=(c == 1))
                    sg, sgk = sgr.next()
                    act(k, sg[:], pg[:], AF.Sigmoid, [pgk, "bg"], [sgk], bias=bg[:, n, dc:dc + 1])
                    if n == 0:
                        tt(k, "dve", acc[:], sg[:], pp[:], ALU.mult, [sgk, ppk], [ack])
                    else:
                        tm, tmk = tmr.next()
                        tt(k, "dve", tm[:], sg[:], pp[:], ALU.mult, [sgk, ppk], [tmk])
                        if n < 3:
                            tt(k, "pool", acc[:], acc[:], tm[:], ALU.add, [ack, tmk], [ack])
                        else:
                            tt(k, "pool", mT[:, dc, tsl], acc[:], tm[:], ALU.add, [ack, tmk], ["mT%d_%d" % (dc, tb)])
        for i in range(NT):
            xt, xk = xr.next()
            k.dma("sp", xt[:], S["Xd"][i * 128:(i + 1) * 128, :], reads=["Xd%d" % i], writes=[xk])
            for dh in range(2):
                po, pok = por.next()
                for dc in range(8):
                    mm(k, po[:], mT[:, dc, i * 128:(i + 1) * 128], wout[:, dc, dh * 512:(dh + 1) * 512], dc == 0, dc == 7,
                       ["mT%d_%d" % (dc, i // 4), "wout"], [pok], inc=(dc == 7))
                tt(k, "dve", xt[:, dh * 512:(dh + 1) * 512], xt[:, dh * 512:(dh + 1) * 512], po[:], ALU.add, [xk, pok], [xk])
            k.dma("sp", S["Xd"][i * 128:(i + 1) * 128, :], xt[:], reads=[xk], writes=["Xd%d" % i])


def phase_moe(nc, k, C, I, S, l):
    CAP = 256
    with Ctx(nc, k, "moe%d" % l) as cx:
        h2 = cx.sb([128, NT, D], BF16, "h2")
        aff = cx.sb([128, NT, 16], F32, "aff")
        posT = cx.sb([128, NT, 16], F32, "posT")
        with Ctx(nc, k, "moeR%d" % l) as c1:
            gt = bcast_load(k, c1, I["norm2_g"][l], D, "g")
            wr = c1.sb([128, 8, 16], F32, "wr")
            k.dma("sp", wr[:], I["w_router"][l].rearrange("(c p) e -> p c e", p=128), writes=["wr"])
            xr = c1.rot_sb(2, [128, D], F32, "x"); hfr = c1.rot_sb(2, [128, D], F32, "hf")
            hfTr = c1.rot_sb(2, [128, 8, 128], F32, "hfT")
            ssr = c1.rot_sb(2, [128, 1], F32, "ss")
            junk = c1.sb([128, D], F32, "junk")
            ptr = c1.rot_ps(2, [128, 4, 128], F32, "pt")
            plr = c1.rot_ps(2, [128, 16], F32, "pl")
            smr = c1.rot_sb(6, [128, 1], F32, "sm")
            exr = c1.rot_sb(2, [128, 16], F32, "ex")
            for i in range(NT):
                xt, xk = xr.next()
                k.dma("sp", xt[:], S["Xd"][i * 128:(i + 1) * 128, :], reads=["Xd%d" % i], writes=[xk])
                ss, sk = ssr.next()
                rms_rstd(k, xt[:], xk, junk[:], "junk", ss[:], sk, D)
                hf, hfk = hfr.next()
                stt(k, hf[:], xt[:], ss[:, 0:1], gt[:], ALU.mult, ALU.mult, [xk, sk, "g"], [hfk])
                cp(k, "act", h2[:, i, :], hf[:], [hfk], ["h2_%d" % i])
                hfT, hfTk = hfTr.next()
                for g in range(2):
                    pt, ptk = ptr.next()
                    for j in range(4):
                        c = g * 4 + j
                        tp(k, pt[:, j, :], hf[:, c * 128:(c + 1) * 128], C["identf"][:], [hfk, "identf"], [ptk], inc=(j == 3))
                    cp(k, "dve" if g else "act", hfT[:, g * 4:(g + 1) * 4, :], pt[:], [ptk], [hfTk])
                pl, plk = plr.next()
                for c in range(8):
                    mm(k, pl[:], hfT[:, c, :], wr[:, c, :], c == 0, c == 7, [hfTk, "wr"], [plk], inc=(c == 7))
                mx, mxk = smr.next(); sm, smk = smr.next(); rs, rsk = smr.next()
                k.op("dve", lambda e: e.reduce_max(out=mx[:], in_=pl[:], axis=AX.X), [plk], [mxk])
                ts(k, "dve", mx[:], mx[:], -1.0, None, ALU.mult, None, [mxk], [mxk])
                ex, exk = exr.next()
                act(k, ex[:], pl[:], AF.Exp, [plk, mxk], [exk, smk], bias=mx[:, 0:1], accum_out=sm[:])
                k.op("dve", lambda e: e.reciprocal(out=rs[:], in_=sm[:]), [smk], [rsk])
                ts(k, "dve", aff[:, i, :], ex[:], rs[:, 0:1], None, ALU.mult, None, [exk, rsk], ["aff%d" % i])
            affT = c1.sb([16, L], F32, "affT"); work = c1.sb([16, L], F32, "work")
            for g in range(4):
                pt, ptk = ptr.next()
                for j in range(4):
                    i = g * 4 + j
                    tp(k, pt[0:16, j, :], aff[:, i, :], C["identf"][:], ["aff%d" % i, "identf"], [ptk], inc=(j == 3))
                cp(k, "act", affT[:, g * 512:(g + 1) * 512], pt[0:16, :, :], [ptk], ["affT"])
            lo = c1.sb([16, 1], F32, "lo"); hi = c1.sb([16, 1], F32, "hi"); mid = c1.sb([16, 1], F32, "mid")
            cnt = c1.sb([16, 1], F32, "cnt"); ge = c1.sb([16, 1], F32, "ge"); dd = c1.sb([16, 1], F32, "dd")
            k.op("dve", lambda e: e.memset(lo[:], 0.0), [], ["lo"])
            k.op("dve", lambda e: e.memset(hi[:], 1.0), [], ["hi"])
            for it in range(30):
                tt(k, "dve", mid[:], lo[:], hi[:], ALU.add, ["lo", "hi"], ["mid"])
                ts(k, "dve", mid[:], mid[:], 0.5, None, ALU.mult, None, ["mid"], ["mid"])
                k.op("dve", lambda e: e.tensor_scalar(out=work[:], in0=affT[:], scalar1=mid[:, 0:1], scalar2=0.0, op0=ALU.is_ge, op1=ALU.add, accum_out=cnt[:]),
                     ["affT", "mid"], ["work", "cnt"])
                ts(k, "dve", ge[:], cnt[:], float(CAP), None, ALU.is_ge, None, ["cnt"], ["ge"])
                tt(k, "dve", dd[:], mid[:], lo[:], ALU.subtract, ["mid", "lo"], ["dd"])
                stt(k, lo[:], dd[:], ge[:, 0:1], lo[:], ALU.mult, ALU.add, ["dd", "ge", "lo"], ["lo"])
                tt(k, "dve", dd[:], hi[:], mid[:], ALU.subtract, ["hi", "mid"], ["dd"])
                stt(k, hi[:], dd[:], ge[:, 0:1], mid[:], ALU.mult, ALU.add, ["dd", "ge", "mid"], ["hi"])
            mask = c1.sb([16, L], F32, "mask"); ones = c1.sb([16, L], F32, "ones"); pos = c1.sb([16, L], F32, "pos")
            k.op("pool", lambda e: e.memset(ones[:], 1.0), [], ["ones"])
            ts(k, "dve", mask[:], affT[:], lo[:, 0:1], None, ALU.is_ge, None, ["affT", "lo"], ["mask"])
            k.op("dve", lambda e: e.tensor_tensor_scan(out=pos[:], data0=ones[:], data1=mask[:], initial=0.0, op0=ALU.mult, op1=ALU.add), ["ones", "mask"], ["pos"])
            tt(k, "dve", pos[:], pos[:], mask[:], ALU.mult, ["pos", "mask"], ["pos"])
            ts(k, "dve", pos[:], pos[:], -1.0, None, ALU.add, None, ["pos"], ["pos"])
            ppos = c1.rot_ps(1, [128, NT, 16], F32, "ppos")
            pp_, ppk = ppos.next()
            for i in range(NT):
                tp(k, pp_[:, i, :], pos[:, i * 128:(i + 1) * 128], C["identf"][0:16, 0:16], ["pos", "identf"], [ppk], inc=(i == NT - 1))
            cp(k, "act", posT[:], pp_[:], [ppk], ["posT"])
            sel = c1.sb([16, 16, 128], BF16, "sel")
            k.dma("pool", sel[:], I["sel16"], writes=["sel"])
            posb = c1.sb([16, L], BF16, "posb")
            cp(k, "act", posb[:], pos[:], ["pos"], ["posb"])
            iotap = c1.sb([128, 2], F32, "iotap")
            k.dma("sp", iotap[:], I["iotap"], writes=["iotap"])
            pbr = c1.rot_ps(2, [128, 512], F32, "pb")
            str_ = c1.rot_sb(4, [128, L], BF16, "st")
            for e in range(16):
                st0, st0k = str_.next(); st1, st1k = str_.next()
                for tb in range(4):
                    pb, pbk = pbr.next()
                    mm(k, pb[:], sel[:, e, :], posb[:, tb * 512:(tb + 1) * 512], True, True, ["sel", "posb"], [pbk])
                    ts(k, "dve", st0[:, tb * 512:(tb + 1) * 512], pb[:], iotap[:, 0:1], None, ALU.is_equal, None, [pbk, "iotap"], [st0k])
                    ts(k, "dve", st1[:, tb * 512:(tb + 1) * 512], pb[:], iotap[:, 1:2], None, ALU.is_equal, None, [pbk, "iotap"], [st1k])
                k.dma("sp", S["ST_d"][e, 0:128, :], st0[:], reads=[st0k], writes=["ST_d%d" % e])
                k.dma("sp", S["ST_d"][e, 128:256, :], st1[:], reads=[st1k], writes=["ST_d%d" % e])
        with Ctx(nc, k, "moeF%d" % l) as c2:
            iota = c2.sb([128, 256], F32, "iota")
            k.dma("sp", iota[:], I["iota256"], writes=["iota"])
            affhl = c2.sb([128, NT, 16, 2], BF16, "affhl")
            afft = c2.sb([128, NT, 16], F32, "afft")
            allaff = ["aff%d" % i for i in range(NT)]
            cp(k, "dve", affhl[:, :, :, 0], aff[:], allaff, ["affhl"])
            cp(k, "dve", afft[:], affhl[:, :, :, 0], ["affhl"], ["afft"])
            tt(k, "dve", afft[:], aff[:], afft[:], ALU.subtract, allaff + ["afft"], ["afft"])
            cp(k, "dve", affhl[:, :, :, 1], afft[:], ["afft"], ["affhl"])
            wgr = c2.rot_sb(2, [128, 8, D], BF16, "wg"); wur = c2.rot_sb(2, [128, 8, D], BF16, "wu"); wdr = c2.rot_sb(2, [128, 8, D], BF16, "wd")
            ser = c2.rot_sb(2, [128, NT, 256], BF16, "se")
            xer = c2.rot_sb(2, [128, 8, 256], BF16, "xe")
            acr = c2.rot_sb(2, [128, 8, 256], BF16, "ac")
            yer = c2.rot_sb(2, [128, 2, D], BF16, "ye")
            asr = c2.rot_sb(2, [128, 2], F32, "as")
            as2r = c2.rot_sb(2, [128, 2, 2], F32, "as2")
            sgr = c2.rot_sb(2, [128, 256], F32, "sg")
            pxr = c2.rot_ps(2, [128, 256], F32, "px")
            par = c2.rot_ps(1, [128, 2, 2], F32, "pa")
            pgr = c2.rot_ps(2, [128, 2, 256], F32, "pgu")
            pyr = c2.rot_ps(2, [128, 512], F32, "py")
            allh2 = ["h2_%d" % i for i in range(NT)]
            def load_ew(e):
                wg, wgk = wgr.next(); wu, wuk = wur.next(); wd, wdk = wdr.next()
                for (wt_, wk_, nm) in ((wg, wgk, "w_e_gate"), (wu, wuk, "w_e_up"), (wd, wdk, "w_e_down")):
                    k.dma("pool", wt_[:], I[nm][l, e].rearrange("(c p) f -> p c f", p=128), writes=[wk_])
                return wg, wgk, wu, wuk, wd, wdk

            nxt = load_ew(0)
            for e in range(16):
                wg, wgk, wu, wuk, wd, wdk = nxt
                if e + 1 < 16:
                    nxt = load_ew(e + 1)
                se, sek = ser.next()
                for c in range(NT):
                    ts(k, "dve", se[:, c, :], iota[:], posT[:, c, e:e + 1], None, ALU.is_equal, None, ["iota", "posT"], [sek])
                xe, xek = xer.next()
                for dk in range(8):
                    px, pxk = pxr.next()
                    for c in range(NT):
                        mm(k, px[:], h2[:, c, dk * 128:(dk + 1) * 128], se[:, c, :], c == 0, c == NT - 1, [allh2[c], sek], [pxk], inc=(c == NT - 1))
                    cp(k, "act" if dk % 2 else "dve", xe[:, dk, :], px[:], [pxk], [xek])
                pa, pak = par.next()
                for jc in range(2):
                    for c in range(NT):
                        mm(k, pa[:, jc, :], se[:, c, jc * 128:(jc + 1) * 128], affhl[:, c, e, :], c == 0, c == NT - 1, [sek, "affhl"], [pak], inc=(c == NT - 1))
                as_, ask = asr.next()
                as2, as2k = as2r.next()
                cp(k, "act", as2[:], pa[:], [pak], [as2k])
                tt(k, "dve", as_[:], as2[:, :, 0], as2[:, :, 1], ALU.add, [as2k], [ask])
                ac, ack = acr.next()
                for f in range(8):
                    pgu, pgk = pgr.next()
                    for dk in range(8):
                        mm(k, pgu[:, 0, :], wg[:, dk, f * 128:(f + 1) * 128], xe[:, dk, :], dk == 0, dk == 7, [wgk, xek], [pgk], inc=False)
                    for dk in range(8):
                        mm(k, pgu[:, 1, :], wu[:, dk, f * 128:(f + 1) * 128], xe[:, dk, :], dk == 0, dk == 7, [wuk, xek], [pgk], inc=(dk == 7))
                    sg, sgk = sgr.next()
                    act(k, sg[:], pgu[:, 0, :], AF.Silu, [pgk], [sgk])
                    tt(k, "dve", ac[:, f, :], sg[:], pgu[:, 1, :], ALU.mult, [sgk, pgk], [ack])
                ye, yek = yer.next()
                for jc in range(2):
                    for dh in range(2):
                        py, pyk = pyr.next()
                        for f in range(8):
                            mm(k, py[:], ac[:, f, jc * 128:(jc + 1) * 128], wd[:, f, dh * 512:(dh + 1) * 512], f == 0, f == 7, [ack, wdk], [pyk], inc=(f == 7))
                        ts(k, "dve", ye[:, jc, dh * 512:(dh + 1) * 512], py[:], as_[:, jc:jc + 1], None, ALU.mult, None, [pyk, ask], [yek])
                k.dma("sp", S["ye_d"][e].rearrange("(jc p) d -> p jc d", p=128), ye[:], reads=[yek], writes=["ye_d%d" % e])
        with Ctx(nc, k, "moeS%d" % l) as c3:
            yeall = c3.sb([128, 32, D], BF16, "yeall")
            for e in range(16):
                k.dma("act" if e % 2 else "sp", yeall[:, 2 * e:2 * e + 2, :], S["ye_d"][e].rearrange("(jc p) d -> p jc d", p=128), reads=["ye_d%d" % e], writes=["yeall%d" % e])
            STr = c3.rot_sb(2, [128, 32, 512], BF16, "ST")
            xr = c3.rot_sb(2, [128, D], F32, "x")
            por = c3.rot_ps(2, [128, 512], F32, "po")
            def load_ST(tb):
                ST, STk = STr.next()
                for e in range(16):
                    k.dma("act" if e % 2 else "sp", ST[:, 2 * e:2 * e + 2, :], S["ST_d"][e, :, tb * 512:(tb + 1) * 512].rearrange("(jc p) t -> p jc t", p=128),
                          reads=["ST_d%d" % e], writes=["%s_%d" % (STk, e)])
                return ST, STk

            nxt = load_ST(0)
            for tb in range(4):
                ST, STk = nxt
                if tb + 1 < 4:
                    nxt = load_ST(tb + 1)
                for j in range(4):
                    i = tb * 4 + j
                    xt, xk = xr.next()
                    k.dma("sp", xt[:], S["Xd"][i * 128:(i + 1) * 128, :], reads=["Xd%d" % i], writes=[xk])
                    for dh in range(2):
                        po, pok = por.next()
                        for m in range(32):
                            mm(k, po[:], ST[:, m, j * 128:(j + 1) * 128], yeall[:, m, dh * 512:(dh + 1) * 512], m == 0, m == 31, ["%s_%d" % (STk, m // 2), "yeall%d" % (m // 2)], [pok], inc=(m == 31))
                        tt(k, "dve", xt[:, dh * 512:(dh + 1) * 512], xt[:, dh * 512:(dh + 1) * 512], po[:], ALU.add, [xk, pok], [xk])
                    k.dma("sp", S["Xd"][i * 128:(i + 1) * 128, :], xt[:], reads=[xk], writes=["Xd%d" % i])


def phase_ple(nc, k, C, I, S, l, out=None):
    with Ctx(nc, k, "ple%d" % l) as cx:
        h3T = cx.sb([128, 8, L], BF16, "h3T")
        phase_norm_T(nc, k, C, "n3_%d" % l, S["Xd"], lambda i: "Xd%d" % i, I["norm3_g"][l], h3T, lambda i: "h3T%d" % i)
        wpg = cx.sb([128, 8, D], BF16, "wpg"); wpp = cx.sb([128, 2, D], BF16, "wpp")
        for j in range(2):
            k.dma("pool", wpg[:, :, j * 512:(j + 1) * 512], I["w_ple_gate"][l, :, j * 512:(j + 1) * 512].rearrange("(c p) n -> p c n", p=128), writes=["wpg"])
        k.dma("pool", wpp[:], I["w_ple_proj"][l].rearrange("(c p) n -> p c n", p=128), writes=["wpp"])
        pinr = cx.rot_sb(2, [128, 256], F32, "pin"); pbr = cx.rot_sb(2, [128, 256], BF16, "pb")
        pTr = cx.rot_sb(2, [128, 2, 128], BF16, "pT")
        xr = cx.rot_sb(2, [128, D], F32, "x")
        sgr = cx.rot_sb(2, [128, 512], F32, "sg"); tmr = cx.rot_sb(2, [128, 512], F32, "tm")
        ptr = cx.rot_ps(2, [128, 2, 128], BF16, "pt")
        pgr = cx.rot_ps(2, [128, 512], F32, "pg"); ppr = cx.rot_ps(2, [128, 512], F32, "pp")
        if out is not None:
            gtf = bcast_load(k, cx, I["final_g"], D, "gf")
            yr = cx.rot_sb(2, [128, D], F32, "y")
            ssr = cx.rot_sb(2, [128, 1], F32, "ssf")
            junk = cx.sb([128, D], F32, "junkf")
        for i in range(NT):
            pin, pink = pinr.next()
            k.dma("act", pin[:], I["p"][l, i * 128:(i + 1) * 128, :], writes=[pink])
            pb, pbk = pbr.next()
            cp(k, "pool", pb[:], pin[:], [pink], [pbk])
            pt, ptk = ptr.next()
            for c in range(2):
                tp(k, pt[:, c, :], pb[:, c * 128:(c + 1) * 128], C["identb"][:], [pbk, "identb"], [ptk], inc=(c == 1))
            pT, pTk = pTr.next()
            cp(k, "act", pT[:], pt[:], [ptk], [pTk])
            xt, xk = xr.next()
            k.dma("sp", xt[:], S["Xd"][i * 128:(i + 1) * 128, :], reads=["Xd%d" % i], writes=[xk])
            for dh in range(2):
                dsl = slice(dh * 512, (dh + 1) * 512)
                pg, pgk = pgr.next(); pp, ppk = ppr.next()
                for c in range(8):
                    mm(k, pg[:], h3T[:, c, i * 128:(i + 1) * 128], wpg[:, c, dsl], c == 0, c == 7, ["wpg"], [pgk], inc=(c == 7))
                for c in range(2):
                    mm(k, pp[:], pT[:, c, :], wpp[:, c, dsl], c == 0, c == 1, [pTk, "wpp"], [ppk], inc=(c == 1))
                sg, sgk = sgr.next()
                act(k, sg[:], pg[:], AF.Sigmoid, [pgk], [sgk])
                tm, tmk = tmr.next()
                tt(k, "dve", tm[:], sg[:], pp[:], ALU.mult, [sgk, ppk], [tmk])
                tt(k, "pool", xt[:, dsl], xt[:, dsl], tm[:], ALU.add, [xk, tmk], [xk])
            if out is None:
                k.dma("sp", S["Xd"][i * 128:(i + 1) * 128, :], xt[:], reads=[xk], writes=["Xd%d" % i])
            else:
                ss, sk = ssr.next()
                rms_rstd(k, xt[:], xk, junk[:], "junkf", ss[:], sk, D)
                y, yk = yr.next()
                stt(k, y[:], xt[:], ss[:, 0:1], gtf[:], ALU.mult, ALU.mult, [xk, sk, "gf"], [yk])
                k.dma("act", out[i * 128:(i + 1) * 128, :], y[:], reads=[yk], writes=["out%d" % i])


def phase_final(nc, k, C, I, S, out):
    with Ctx(nc, k, "fin") as cx:
        gt = bcast_load(k, cx, I["final_g"], D, "g")
        xr = cx.rot_sb(2, [128, D], F32, "x"); yr = cx.rot_sb(2, [128, D], F32, "y")
        ssr = cx.rot_sb(2, [128, 1], F32, "ss")
        junk = cx.sb([128, D], F32, "junk")
        for i in range(NT):
            xt, xk = xr.next()
            k.dma("sp", xt[:], S["Xd"][i * 128:(i + 1) * 128, :], reads=["Xd%d" % i], writes=[xk])
            ss, sk = ssr.next()
            rms_rstd(k, xt[:], xk, junk[:], "junk", ss[:], sk, D)
            y, yk = yr.next()
            stt(k, y[:], xt[:], ss[:, 0:1], gt[:], ALU.mult, ALU.mult, [xk, sk, "g"], [yk])
            k.dma("act", out[i * 128:(i + 1) * 128, :], y[:], reads=[yk], writes=["out%d" % i])


def build_program(dbg=False, nlayers=DEPTH, phases=None, track=None):
    bf = ml_dtypes.bfloat16
    nc = bass.Bass("TRN2", target_bir_lowering=False)
    k = KB(nc)
    k.track = track
    I = {}

    def din(name, shape, dt=F32):
        I[name] = nc.dram_tensor(name, list(shape), dt, kind="ExternalInput").ap()

    din("x", (L, D)); din("p", (2, L, 256))
    for n, s in WEIGHT_SHAPES.items():
        din(n, s)
    for n, v in const_specs().items():
        din(n, v.shape, BF16 if v.dtype == bf else F32)
    out = nc.dram_tensor("out", [L, D], F32, kind="ExternalOutput").ap()
    S = {}

    def dscr(name, shape, dt):
        S[name] = nc.dram_tensor(name, list(shape), dt, kind=("ExternalOutput" if dbg else "Internal")).ap()

    dscr("Xd", (L, D), F32)
    dscr("hTd", (128, 8, L), BF16)
    dscr("Yd", (4, 256, L), BF16)
    dscr("x1Td", (256, L), F32); dscr("x2Td", (256, L), F32); dscr("vTd", (256, L), F32)
    dscr("v_d", (L, 256), BF16); dscr("z2Td", (256, L), F32); dscr("z2_d", (L, 256), BF16)
    dscr("fn_d", (2, L, 256), BF16)
    dscr("KAd", (2, L, 256), F32); dscr("KBd", (2, L, 256), F32); dscr("KNd", (2, 256), F32)
    dscr("bias_d", (60, 4096), F32)
    dscr("ye_d", (16, 256, D), BF16); dscr("ST_d", (16, 256, L), BF16)

    def on(ph):
        return phases is None or ph in phases

    with Ctx(nc, k, "glob") as g:
        C = {}
        C["identf"] = g.sb([128, 128], F32, "identf")
        C["identb"] = g.sb([128, 128], BF16, "identb")
        k.dma("sp", C["identf"][:], I["ident"], writes=["identf"])
        cp(k, "dve", C["identb"][:], C["identf"][:], ["identf"], ["identb"])
        k.dma("sp", S["Xd"], I["x"], writes=["Xd%d" % i for i in range(NT)])
        k.barrier()
        for l in range(nlayers):
            with Ctx(nc, k, "mix%d" % l) as mx:
                hT = mx.sb([128, 8, L], BF16, "hT")
                phase_norm_T(nc, k, C, "n1_%d" % l, S["Xd"], lambda i: "Xd%d" % i, I["norm1_g"][l], hT, lambda i: "hT%d" % i)
                if on("mla"):
                    phase_mla(nc, k, C, I, S, l, hT)
                if on("na"):
                    phase_na(nc, k, C, I, S, l, hT)
                if on("hy"):
                    phase_hyfn_prep(nc, k, C, I, S, l, hT)
                k.dma("sp", S["hTd"], hT[:], reads=["hT%d" % i for i in range(NT)], writes=["hTd"])
            if on("hy"):
                phase_dft(nc, k, C, I, S, l)
            if on("gate"):
                phase_gate(nc, k, C, I, S, l)
            if on("moe"):
                phase_moe(nc, k, C, I, S, l)
            fuse_final = on("final") and on("ple") and l == nlayers - 1
            if on("ple"):
                phase_ple(nc, k, C, I, S, l, out if fuse_final else None)
        if on("final") and not on("ple"):
            phase_final(nc, k, C, I, S, out)
        k.finish("sp")
    return nc


def make_in_maps(inputs, cores):
    consts = const_specs()
    maps = []
    for b in cores:
        m = {"x": np.ascontiguousarray(inputs["x"][b]), "p": np.ascontiguousarray(inputs["p"][:, b])}
        for n in WEIGHT_SHAPES:
            m[n] = np.ascontiguousarray(inputs[n])
        m.update(consts)
        maps.append(m)
    return maps


def kernel(**inputs):
    inputs = {k_: np.asarray(v) for k_, v in inputs.items()}
    nc = build_program()
    maps = make_in_maps(inputs, list(range(8)))
    res = run_bass_kernel_spmd(nc, maps, core_ids=list(range(8)))
    return np.stack([np.asarray(r["out"]) for r in res.results], axis=0).astype(np.float32)
```
